# Optimizing a Trainium2 kernel written in Bass

```python
import jax, jax.numpy as jnp
from jax import lax
import numpy as np

D_MODEL = 2048
BATCH = 8
SEQ = 2048
DEPTH = 2

HEAD_DIM = 128
GDN_HEADS = 8
SB_HEADS = 8
GDN_CONV = 4
GDN_CHUNK = 64
ATTN_HEADS = 16
ATTN_KV_HEADS = 4
IDX_HEADS = 16
IDX_DIM = 64
TOPK_MAX = 256
TOPK_KEY_FRACTION = 4
Q_BLOCK = 128
ROPE_THETA = 500000.0
ROPE_FRACTION = 4
D_FF = -(-8 * D_MODEL // (3 * 256)) * 256
NORM_EPS = 1e-6

GDN_WIDTH = GDN_HEADS * HEAD_DIM
SB_WIDTH = SB_HEADS * HEAD_DIM
AB_WIDTH = GDN_WIDTH + SB_WIDTH
AB_SPLITS = (3 * GDN_WIDTH, GDN_WIDTH, GDN_HEADS, GDN_HEADS, SB_WIDTH, SB_WIDTH, SB_WIDTH)
AB_IN = sum(AB_SPLITS)
C_Q = ATTN_HEADS * HEAD_DIM
C_KV = ATTN_KV_HEADS * HEAD_DIM
C_SPLITS = (C_Q, C_KV, C_KV, IDX_HEADS * IDX_DIM, IDX_DIM, IDX_HEADS)
C_IN = sum(C_SPLITS)
N_EVEN = (DEPTH + 1) // 2
N_ODD = DEPTH // 2

kernel_name = "hybrid_gdn_stickbreak_dsa_block"


def rmsnorm(x, g):
    xf = x.astype(jnp.float32)
    y = xf * lax.rsqrt(jnp.mean(xf * xf, axis=-1, keepdims=True) + NORM_EPS)
    return (y * g.astype(jnp.float32)).astype(x.dtype)


def l2norm(x):
    xf = x.astype(jnp.float32)
    return xf * lax.rsqrt(jnp.sum(xf * xf, axis=-1, keepdims=True) + NORM_EPS)


def split_cols(t, sizes):
    offs = np.cumsum(sizes)[:-1]
    return jnp.split(t, [int(o) for o in offs], axis=-1)


def partial_rope(x, positions):
    d = x.shape[-1]
    r = d // ROPE_FRACTION
    half = r // 2
    inv_freq = ROPE_THETA ** (-jnp.arange(half, dtype=jnp.float32) / half)
    ang = positions.astype(jnp.float32)[..., None] * inv_freq
    cos = jnp.cos(ang)[:, :, None, :]
    sin = jnp.sin(ang)[:, :, None, :]
    xf = x.astype(jnp.float32)
    x1, x2, rest = xf[..., :half], xf[..., half:r], xf[..., r:]
    out = jnp.concatenate([x1 * cos - x2 * sin, x2 * cos + x1 * sin, rest], axis=-1)
    return out.astype(x.dtype)


def causal_depthwise_conv(x, w):
    K, C = w.shape
    return lax.conv_general_dilated(
        x, w[:, None, :].astype(x.dtype), window_strides=(1,), padding=[(K - 1, 0)],
        dimension_numbers=("NWC", "WIO", "NWC"), feature_group_count=C)


def gated_delta_rule_chunked(q, k, v, g, beta):
    B, S, H, dk = q.shape
    dv = v.shape[-1]
    C = GDN_CHUNK
    N = S // C
    f32 = jnp.float32

    def chunks(t):
        return t.astype(f32).reshape(B, N, C, H, -1).transpose(1, 0, 3, 2, 4)

    qc = chunks(q) * (dk ** -0.5)
    kc = chunks(k)
    vc = chunks(v)
    gc = g.astype(f32).reshape(B, N, C, H).transpose(1, 0, 3, 2)
    bc = beta.astype(f32).reshape(B, N, C, H).transpose(1, 0, 3, 2)
    gcum = jnp.cumsum(gc, axis=-1)
    tril_incl = jnp.tril(jnp.ones((C, C), dtype=bool))
    tril_strict = jnp.tril(jnp.ones((C, C), dtype=bool), -1)
    decay = jnp.exp(jnp.where(tril_incl, gcum[..., :, None] - gcum[..., None, :], -jnp.inf))
    kb = kc * bc[..., None]
    lower = jnp.where(tril_strict, jnp.einsum('nbhid,nbhjd->nbhij', kb, kc) * decay, 0.0)
    eye = jnp.eye(C, dtype=f32)
    rhs = jnp.concatenate([vc * bc[..., None], kb * jnp.exp(gcum)[..., None]], axis=-1)
    sol = lax.linalg.triangular_solve(eye + lower, rhs, left_side=True, lower=True,
                                      unit_diagonal=True)
    u, w = sol[..., :dv], sol[..., dv:]
    intra = jnp.einsum('nbhid,nbhjd->nbhij', qc, kc) * decay

    def step(state, inp):
        q_i, k_i, u_i, w_i, a_i, g_i = inp
        v_new = u_i - jnp.einsum('bhcd,bhde->bhce', w_i, state)
        o = (jnp.einsum('bhcd,bhde->bhce', q_i * jnp.exp(g_i)[..., None], state)
             + jnp.einsum('bhij,bhje->bhie', a_i, v_new))
        g_last = g_i[..., -1]
        k_dec = k_i * jnp.exp(g_last[..., None] - g_i)[..., None]
        state = state * jnp.exp(g_last)[..., None, None] + jnp.einsum('bhcd,bhce->bhde', k_dec, v_new)
        return state, o

    state0 = jnp.zeros((B, H, dk, dv), f32)
    _, o = lax.scan(step, state0, (qc, kc, u, w, intra, gcum))
    return o.transpose(1, 0, 3, 2, 4).reshape(B, S, H, dv)


def stick_breaking_attention(q, k, v):
    B, S, H, d = q.shape
    scale = d ** -0.5
    outs = []
    for blk in range(S // Q_BLOCK):
        q0 = blk * Q_BLOCK
        kv_len = q0 + Q_BLOCK
        qb = q[:, q0:kv_len].astype(jnp.float32)
        kb = k[:, :kv_len].astype(jnp.float32)
        vb = v[:, :kv_len].astype(jnp.float32)
        z = jnp.einsum('bqhd,bkhd->bhqk', qb, kb) * scale
        t_idx = q0 + jnp.arange(Q_BLOCK)[:, None]
        s_idx = jnp.arange(kv_len)[None, :]
        causal = s_idx < t_idx
        log_stay = jnp.where(causal, jax.nn.log_sigmoid(-z), 0.0)
        between = lax.cumsum(log_stay, axis=3, reverse=True) - log_stay
        a = jnp.where(causal, jnp.exp(jax.nn.log_sigmoid(z) + between), 0.0)
        outs.append(jnp.einsum('bhqk,bkhd->bqhd', a, vb))
    return jnp.concatenate(outs, axis=1).astype(q.dtype)


def dsa_sparse_attention(q, k, v, qi, ki, wi):
    B, S, H, d = q.shape
    Hkv = k.shape[2]
    G = H // Hkv
    topk = min(TOPK_MAX, S // TOPK_KEY_FRACTION)
    nb = S // Q_BLOCK
    scale = d ** -0.5
    kf = k.astype(jnp.float32)
    vf = v.astype(jnp.float32)
    kif = ki.astype(jnp.float32)

    def blockify(t):
        return t.reshape(B, nb, Q_BLOCK, *t.shape[2:]).swapaxes(0, 1)

    def one_block(args):
        blk, qb, qib, wib = args
        t_idx = blk * Q_BLOCK + jnp.arange(Q_BLOCK)
        causal = jnp.arange(S)[None, :] <= t_idx[:, None]
        idx_logits = jnp.einsum('bqhe,bse->bqhs', qib.astype(jnp.float32), kif)
        score = jnp.einsum('bqh,bqhs->bqs', wib.astype(jnp.float32), jax.nn.relu(idx_logits))
        score = jnp.where(causal[None], score, -jnp.inf)
        _, sel = lax.top_k(score, topk)
        valid = sel <= t_idx[None, :, None]
        k_sel = jax.vmap(lambda kk, ii: kk[ii])(kf, sel)
        v_sel = jax.vmap(lambda vv, ii: vv[ii])(vf, sel)
        qg = qb.astype(jnp.float32).reshape(B, Q_BLOCK, Hkv, G, d)
        logits = jnp.einsum('bqhgd,bqkhd->bqhgk', qg, k_sel) * scale
        logits = jnp.where(valid[:, :, None, None, :], logits, -jnp.inf)
        p = jax.nn.softmax(logits, axis=-1)
        o = jnp.einsum('bqhgk,bqkhd->bqhgd', p, v_sel)
        return o.reshape(B, Q_BLOCK, H, d)

    out = lax.map(one_block, (jnp.arange(nb), blockify(q), blockify(qi), blockify(wi)))
    return out.swapaxes(0, 1).reshape(B, S, H, d).astype(q.dtype)


def mixer_ab(h, w_in, conv_w, a_log, dt_bias, gdn_norm, w_out):
    B, S, _ = h.shape
    proj = h @ w_in
    a_qkv, a_gate, a_alpha, a_beta, b_q, b_k, b_v = split_cols(proj, AB_SPLITS)
    a_qkv = jax.nn.silu(causal_depthwise_conv(a_qkv, conv_w))
    aq, ak, av = [t.reshape(B, S, GDN_HEADS, HEAD_DIM) for t in jnp.split(a_qkv, 3, axis=-1)]
    beta = jax.nn.sigmoid(a_beta.astype(jnp.float32))
    g = -jnp.exp(a_log.astype(jnp.float32)) * jax.nn.softplus(
        a_alpha.astype(jnp.float32) + dt_bias.astype(jnp.float32))
    o_a = gated_delta_rule_chunked(l2norm(aq), l2norm(ak), av, g, beta).astype(h.dtype)
    o_a = rmsnorm(o_a, gdn_norm) * jax.nn.silu(a_gate.reshape(B, S, GDN_HEADS, HEAD_DIM))
    o_b = stick_breaking_attention(b_q.reshape(B, S, SB_HEADS, HEAD_DIM),
                                   b_k.reshape(B, S, SB_HEADS, HEAD_DIM),
                                   b_v.reshape(B, S, SB_HEADS, HEAD_DIM))
    o = jnp.concatenate([o_a.reshape(B, S, GDN_WIDTH), o_b.reshape(B, S, SB_WIDTH)], axis=-1)
    return o @ w_out


def mixer_c(h, positions, w_in, w_out):
    B, S, _ = h.shape
    proj = h @ w_in
    q, k, v, qi, ki, wi = split_cols(proj, C_SPLITS)
    q = partial_rope(q.reshape(B, S, ATTN_HEADS, HEAD_DIM), positions)
    k = partial_rope(k.reshape(B, S, ATTN_KV_HEADS, HEAD_DIM), positions)
    v = v.reshape(B, S, ATTN_KV_HEADS, HEAD_DIM)
    qi = partial_rope(qi.reshape(B, S, IDX_HEADS, IDX_DIM), positions)
    ki = partial_rope(ki.reshape(B, S, 1, IDX_DIM), positions)[:, :, 0]
    wi = wi * ((IDX_HEADS ** -0.5) * (IDX_DIM ** -0.5))
    o = dsa_sparse_attention(q, k, v, qi, ki, wi)
    return o.reshape(B, S, C_Q) @ w_out


def swiglu(h, w_gate, w_up, w_down):
    return (jax.nn.silu(h @ w_gate) * (h @ w_up)) @ w_down


def setup_inputs(seed: int = 0) -> dict:
    key = jax.random.key(seed)
    ks = jax.random.split(key, 20)
    f32 = jnp.float32

    def nrm(k, shape, fan_in):
        return jax.random.normal(k, shape, f32) * (fan_in ** -0.5)

    x = jax.random.normal(ks[0], (BATCH, SEQ, D_MODEL), f32)
    offs = jax.random.randint(ks[1], (BATCH, 1), 0, 4096, dtype=jnp.int32)
    positions = offs + jnp.arange(SEQ, dtype=jnp.int32)[None, :]
    norm_mix = 1.0 + 0.02 * jax.random.normal(ks[2], (DEPTH, D_MODEL), f32)
    norm_ffn = 1.0 + 0.02 * jax.random.normal(ks[3], (DEPTH, D_MODEL), f32)
    final_norm = 1.0 + 0.02 * jax.random.normal(ks[4], (D_MODEL,), f32)
    w_in_ab = nrm(ks[5], (N_EVEN, D_MODEL, AB_IN), D_MODEL)
    conv_w_a = nrm(ks[6], (N_EVEN, GDN_CONV, 3 * GDN_WIDTH), GDN_CONV)
    a_log = jnp.log(jax.random.uniform(ks[7], (N_EVEN, GDN_HEADS), f32, 1.0, 16.0))
    dt = jnp.exp(jax.random.uniform(ks[8], (N_EVEN, GDN_HEADS), f32, np.log(1e-3), np.log(1e-1)))
    dt_bias = dt + jnp.log(-jnp.expm1(-dt))
    gdn_norm = 1.0 + 0.02 * jax.random.normal(ks[9], (N_EVEN, HEAD_DIM), f32)
    w_out_ab = nrm(ks[10], (N_EVEN, AB_WIDTH, D_MODEL), AB_WIDTH)
    w_in_c = nrm(ks[11], (N_ODD, D_MODEL, C_IN), D_MODEL)
    w_out_c = nrm(ks[12], (N_ODD, C_Q, D_MODEL), C_Q)
    ffn_gate = nrm(ks[13], (DEPTH, D_MODEL, D_FF), D_MODEL)
    ffn_up = nrm(ks[14], (DEPTH, D_MODEL, D_FF), D_MODEL)
    ffn_down = nrm(ks[15], (DEPTH, D_FF, D_MODEL), D_FF)
    return {"x": x, "positions": positions, "norm_mix": norm_mix, "norm_ffn": norm_ffn,
            "final_norm": final_norm, "w_in_ab": w_in_ab, "conv_w_a": conv_w_a, "a_log": a_log,
            "dt_bias": dt_bias, "gdn_norm": gdn_norm, "w_out_ab": w_out_ab, "w_in_c": w_in_c,
            "w_out_c": w_out_c, "ffn_gate": ffn_gate, "ffn_up": ffn_up, "ffn_down": ffn_down}


def reference(x, positions, norm_mix, norm_ffn, final_norm, w_in_ab, conv_w_a, a_log, dt_bias,
              gdn_norm, w_out_ab, w_in_c, w_out_c, ffn_gate, ffn_up, ffn_down):
    for layer in range(DEPTH):
        h = rmsnorm(x, norm_mix[layer])
        j = layer // 2
        if layer % 2 == 0:
            x = x + mixer_ab(h, w_in_ab[j], conv_w_a[j], a_log[j], dt_bias[j], gdn_norm[j], w_out_ab[j])
        else:
            x = x + mixer_c(h, positions, w_in_c[j], w_out_c[j])
        h = rmsnorm(x, norm_ffn[layer])
        x = x + swiglu(h, ffn_gate[layer], ffn_up[layer], ffn_down[layer])
    return rmsnorm(x, final_norm)
```

```python
import numpy as np
from contextlib import ExitStack
import concourse.bass as bass
import concourse.mybir as mybir
from concourse.bass_utils import run_bass_kernel_spmd

F32 = mybir.dt.float32
BF16 = mybir.dt.bfloat16
I32 = mybir.dt.int32
AF = mybir.ActivationFunctionType
ALU = mybir.AluOpType

S = 2048
D = 2048
DFF = 5632
EPS = 1e-6
NEG = -1.0e30
NEG16 = -60000.0
F16 = mybir.dt.float16
SAME_ENGINE_SYNC = True
N_DMA_SEMS = 24

AB_IN = 7184
C_IN = 4176

C_IDENT = 0
C_ONES = 128
C_SBTRI = 256
C_SBMSK = 384
C_TRI8 = 512
C_ID8 = 1024
C_MS8 = 1536
C_U = 2048
C_CB = 2112
C_RQ = 2240
C_RI = 2368
C_FQ = 2496
C_FI = 2497
NCONST = 2498


def build_consts():
    c = np.zeros((128, NCONST), np.float32)
    p = np.arange(128)[:, None]
    f = np.arange(128)[None, :]
    c[:, C_IDENT:C_IDENT + 128] = (p == f)
    c[:, C_ONES:C_ONES + 128] = 1.0
    c[:, C_SBTRI:C_SBTRI + 128] = (p > f)
    c[:, C_SBMSK:C_SBMSK + 128] = (f > p)
    p64 = np.arange(64)[:, None]
    i64 = np.arange(64)[None, :]
    for h in range(8):
        c[:64, C_TRI8 + h * 64:C_TRI8 + (h + 1) * 64] = (p64 <= i64)
        c[:64, C_ID8 + h * 64:C_ID8 + (h + 1) * 64] = (p64 == i64)
        c[:64, C_MS8 + h * 64:C_MS8 + (h + 1) * 64] = (p64 < i64)
    c[:64, C_U:C_U + 64] = (p64 > i64)
    c[:, C_CB:C_CB + 128] = np.where(f <= p, 0.0, NEG)
    rq = np.zeros((128, 128), np.float32)
    for m in range(16):
        rq[m + 16, m] = -1.0
        rq[m, m + 16] = 1.0
    c[:, C_RQ:C_RQ + 128] = rq
    ri = np.zeros((128, 128), np.float32)
    for base in (0, 64):
        for m in range(8):
            ri[base + m + 8, base + m] = -1.0
            ri[base + m, base + m + 8] = 1.0
    c[:, C_RI:C_RI + 128] = ri
    fq = np.zeros(128, np.float64)
    for m in range(32):
        fq[m] = 500000.0 ** (-(m % 16) / 16.0)
    fi = np.zeros(128, np.float64)
    for base in (0, 64):
        for m in range(16):
            fi[base + m] = 500000.0 ** (-(m % 8) / 8.0)
    c[:, C_FQ] = fq.astype(np.float32)
    c[:, C_FI] = fi.astype(np.float32)
    return c


class Buf:
    __slots__ = ("w", "r")

    def __init__(self):
        self.w = None
        self.r = []


class Tl:
    __slots__ = ("t", "b")

    def __init__(self, t):
        self.t = t
        self.b = Buf()

    def __getitem__(self, k):
        return self.t[k]


ENGS = ("pe", "dve", "act", "pool", "sp")


class KB:
    def __init__(self, nc, es):
        self.nc = nc
        self.sems = []
        self.thunks = {e: [] for e in ENGS}
        self.cnt = {e: 0 for e in ENGS}
        self.waited = {e: {} for e in ENGS}
        self.esid = {}
        for e in ENGS:
            self.esid[e] = len(self.sems)
            self.sems.append(es.enter_context(nc.semaphore("s_" + e)))
        self.dsid = []
        self.dtot = []
        for i in range(N_DMA_SEMS):
            self.dsid.append(len(self.sems))
            self.sems.append(es.enter_context(nc.semaphore("d%d" % i)))
            self.dtot.append(0)
        self.di = 0
        self.ninst = 0

    def _wait(self, e, tok, raw=True):
        if tok is None:
            return
        sid, val = tok
        if sid == self.esid[e] and (e == "pe" or not SAME_ENGINE_SYNC or not raw):
            return
        if self.waited[e].get(sid, 0) >= val:
            return
        self.waited[e][sid] = val
        sem = self.sems[sid]
        self.thunks[e].append(lambda eng: eng.wait_ge(sem, val))

    def _deps(self, e, reads, writes):
        for b in reads:
            self._wait(e, b.w)
        for b in writes:
            self._wait(e, b.w, raw=False)
            for t in b.r:
                self._wait(e, t, raw=False)

    def _commit(self, tok, reads, writes):
        for b in reads:
            b.r.append(tok)
            if len(b.r) > 64:
                b.r = b.r[-48:]
        for b in writes:
            b.w = tok
            b.r = []

    def op(self, e, fn, reads=(), writes=()):
        reads = [x.b if isinstance(x, Tl) else x for x in reads]
        writes = [x.b if isinstance(x, Tl) else x for x in writes]
        self._deps(e, reads, writes)
        self.cnt[e] += 1
        sid = self.esid[e]
        sem = self.sems[sid]
        self.thunks[e].append(lambda eng: fn(eng).then_inc(sem, 1))
        self._commit((sid, self.cnt[e]), reads, writes)
        self.ninst += 1

    def dma(self, q, out, in_, reads=(), writes=()):
        reads = [x.b if isinstance(x, Tl) else x for x in reads]
        writes = [x.b if isinstance(x, Tl) else x for x in writes]
        i = self.di
        self.di = (i + 1) % N_DMA_SEMS
        sid = self.dsid[i]
        if self.dtot[i] > 0:
            self._wait(q, (sid, self.dtot[i]))
        self._deps(q, reads, writes)
        self.dtot[i] += 16
        sem = self.sems[sid]
        self.thunks[q].append(lambda eng: eng.dma_start(out=out, in_=in_).then_inc(sem, 16))
        self._commit((sid, self.dtot[i]), reads, writes)
        self.ninst += 1

    def barrier(self):
        toks = [(self.esid[e], self.cnt[e]) for e in ENGS if self.cnt[e] > 0]
        toks += [(self.dsid[i], self.dtot[i]) for i in range(N_DMA_SEMS) if self.dtot[i] > 0]
        for e in ENGS:
            for t in toks:
                if t[0] != self.esid[e]:
                    self._wait(e, t)

    def mm(self, out, pairs, reads, writes):
        n = len(pairs)

        def fn(eng):
            ins = None
            for i, (l, r) in enumerate(pairs):
                ins = eng.matmul(out, l, r, start=(i == 0), stop=(i == n - 1))
            return ins
        self.op("pe", fn, reads, writes)

    def mm_multi(self, groups, reads, writes):
        def fn(eng):
            ins = None
            for out, pairs in groups:
                n = len(pairs)
                for i, (l, r) in enumerate(pairs):
                    ins = eng.matmul(out, l, r, start=(i == 0), stop=(i == n - 1))
            return ins
        self.op("pe", fn, reads, writes)

    def tr_multi(self, items, ident, reads, writes):
        def fn(eng):
            ins = None
            for out, in_ in items:
                ins = eng.transpose(out, in_, ident)
            return ins
        self.op("pe", fn, reads, writes)

    def act(self, out, in_, func, reads, writes, bias=None, scale=None, accum=None):
        kw = {}
        if bias is not None:
            kw["bias"] = bias
        if scale is not None:
            kw["scale"] = scale
        if accum is not None:
            kw["accum_out"] = accum
        self.op("act", lambda eng: eng.activation(out=out, in_=in_, func=func, **kw), reads, writes)

    def tt(self, out, in0, in1, op, reads, writes, e="dve"):
        self.op(e, lambda eng: eng.tensor_tensor(out=out, in0=in0, in1=in1, op=op), reads, writes)

    def ts(self, out, in0, s1, s2, op0, op1, reads, writes, e="dve"):
        if op1 is None:
            self.op(e, lambda eng: eng.tensor_scalar(out=out, in0=in0, scalar1=s1, scalar2=None, op0=op0), reads, writes)
        else:
            self.op(e, lambda eng: eng.tensor_scalar(out=out, in0=in0, scalar1=s1, scalar2=s2, op0=op0, op1=op1), reads, writes)

    def stt(self, out, in0, scalar, in1, op0, op1, reads, writes):
        self.op("dve", lambda eng: eng.scalar_tensor_tensor(out=out, in0=in0, scalar=scalar, in1=in1, op0=op0, op1=op1), reads, writes)

    def cp(self, e, out, in_, reads, writes):
        if e == "act":
            self.op("act", lambda eng: eng.copy(out=out, in_=in_), reads, writes)
        else:
            self.op(e, lambda eng: eng.tensor_copy(out=out, in_=in_), reads, writes)


class Ctx:
    pass


_UID = [0]


def sb(nc, es, name, shape, dt):
    _UID[0] += 1
    return Tl(es.enter_context(nc.sbuf_tensor("%s_u%d" % (name, _UID[0]), list(shape), dt)))


def phase_norm(g, xsrc, gain_ap, hT):
    kb, nc = g.kb, g.nc
    with ExitStack() as es:
        xs = [sb(nc, es, "nx%d" % i, [128, 16, 512], F32) for i in range(2)]
        sq = sb(nc, es, "nsq", [128, 16, 512], BF16)
        rs = sb(nc, es, "nrs", [128, 512], F32)
        xv = xsrc.rearrange("(c p) s -> p c s", p=128)
        for t in range(4):
            x = xs[t % 2]
            for q4 in range(4):
                kb.dma("sp", x[:, q4 * 4:(q4 + 1) * 4, :], xv[:, q4 * 4:(q4 + 1) * 4, t * 512:(t + 1) * 512], [], [x])
            kb.act(sq[:, :, :], x[:, :, :], AF.Square, [x], [sq])
            ps = g.psum()
            kb.mm(ps[:, :], [(g.onesb[:, :], sq[:, c, :]) for c in range(16)], [sq], [ps])
            kb.act(rs[:, :], ps[:, :], AF.Ln, [ps], [rs], bias=g.eps[:, 0:1], scale=1.0 / D)
            kb.act(rs[:, :], rs[:, :], AF.Exp, [rs], [rs], scale=-0.5)
            for c in range(16):
                kb.stt(hT[:, c, t * 512:(t + 1) * 512], x[:, c, :], gain_ap[:, c:c + 1], rs[:, :],
                       ALU.mult, ALU.mult, [x, rs], [hT])
        kb.barrier()


def dense(g, XT, KC, toks0, ntok, W, blocks, epi, W2=None, NB=512, tm_epi=None):
    kb, nc = g.kb, g.nc
    with ExitStack() as es:
        nw = 2 if W2 is None else 4
        wbs = [sb(nc, es, "wb%d" % i, [128, KC, NB], BF16) for i in range(nw)]
        Wv = W.rearrange("(c p) n -> p c n", p=128)
        W2v = W2.rearrange("(c p) n -> p c n", p=128) if W2 is not None else None
        kstep = 8 if KC <= 16 else 11
        pending = [None]
        blocks = list(blocks)
        if blocks and not blocks[0].get("tm") and len(blocks[0]["segs"]) > 1:
            b0 = blocks[0]
            (off0, w0, tag0) = b0["segs"][0]
            first = {"c0": b0["c0"], "w": w0, "segs": [(0, w0, tag0)]}
            rest = {"c0": b0["c0"] + w0, "w": b0["w"] - w0, "segs": [(o - w0, w, t) for (o, w, t) in b0["segs"][1:]]}
            blocks = [first, rest] + blocks[1:]
        for bi, blk in enumerate(blocks):
            c0, w = blk["c0"], blk["w"]
            if W2 is None:
                wb = wbs[bi % 2]
                wb2 = None
            else:
                wb = wbs[(bi % 2) * 2]
                wb2 = wbs[(bi % 2) * 2 + 1]
            for k0 in range(0, KC, kstep):
                k1 = min(KC, k0 + kstep)
                kb.dma("pool", wb[:, k0:k1, 0:w], Wv[:, k0:k1, c0:c0 + w], [], [wb])
                if wb2 is not None:
                    kb.dma("pool", wb2[:, k0:k1, 0:w], W2v[:, k0:k1, c0:c0 + w], [], [wb2])
            if blk.get("tm"):
                if pending[0] is not None:
                    pending[0]()
                    pending[0] = None
                for tb in range(ntok // 128):
                    ps = g.psum()
                    kb.mm(ps[:, 0:w], [(XT[:, c, tb * 128:(tb + 1) * 128], wb[:, c, 0:w]) for c in range(KC)],
                          [XT, wb], [ps])
                    tm_epi(c0, w, toks0 + tb * 128, ps)
            else:
                for tt in range(ntok // 512):
                    for (off, width, tag) in blk["segs"]:
                        ps = g.psum()
                        kb.mm(ps[0:width, :], [(wb[:, c, off:off + width], XT[:, c, tt * 512:(tt + 1) * 512]) for c in range(KC)],
                              [XT, wb], [ps])
                        ps2 = None
                        if wb2 is not None:
                            ps2 = g.psum()
                            kb.mm(ps2[0:width, :], [(wb2[:, c, off:off + width], XT[:, c, tt * 512:(tt + 1) * 512]) for c in range(KC)],
                                  [XT, wb2], [ps2])
                        if pending[0] is not None:
                            pending[0]()
                        pending[0] = epi(tag, c0 + off, width, toks0 + tt * 512, ps, ps2)
        if pending[0] is not None:
            pending[0]()
        kb.barrier()


def make_blocks(segments, NB=512):
    blocks = []
    cur = None
    for (c0, w, tag, tm) in segments:
        if tm:
            blocks.append({"c0": c0, "w": w, "tm": True})
            cur = None
            continue
        if cur is not None and cur["c0"] + cur["w"] == c0 and cur["w"] + w <= NB:
            cur["segs"].append((cur["w"], w, tag))
            cur["w"] += w
        else:
            cur = {"c0": c0, "w": w, "segs": [(0, w, tag)]}
            blocks.append(cur)
    return blocks


class Stager:
    def __init__(self, g, es, name, shape, dt, n=3):
        self.tiles = [sb(g.nc, es, "%s%d" % (name, i), shape, dt) for i in range(n)]
        self.i = 0

    def next(self):
        t = self.tiles[self.i % len(self.tiles)]
        self.i += 1
        return t


def phase_residual_dense(g, XT, KC, toks0, ntok, W, xres, NB):
    kb, nc = g.kb, g.nc
    with ExitStack() as es:
        xin = Stager(g, es, "rxin", [128, 512], F32, 3)
        xout = Stager(g, es, "rxo", [128, 512], F32, 3)

        def epi(tag, c, width, t0, ps, ps2):
            a = xin.next()
            kb.dma("sp", a[:, :], xres[c:c + 128, t0:t0 + 512], [], [a])
            o = xout.next()
            kb.tt(o[:, :], ps[:, :], a[:, :], ALU.add, [ps, a], [o])
            kb.dma("sp", xres[c:c + 128, t0:t0 + 512], o[:, :], [o], [])
        segs = [(c * 128, 128, None, False) for c in range(D // 128)]
        dense(g, XT, KC, toks0, ntok, W, make_blocks(segs, NB), epi, NB=NB)


def phase_ffn(g, layer, hT):
    kb, nc = g.kb, g.nc
    Wg = g.ffn_gate[layer * D:(layer + 1) * D, :]
    Wu = g.ffn_up[layer * D:(layer + 1) * D, :]
    Wd = g.ffn_down[layer * DFF:(layer + 1) * DFF, :]
    with ExitStack() as es:
        sg = Stager(g, es, "fsg", [128, 512], F32, 2)
        st = Stager(g, es, "fst", [128, 512], BF16, 3)

        def epi(tag, c, width, t0, ps, ps2):
            a = sg.next()
            kb.act(a[:, :], ps[:, :], AF.Silu, [ps], [a])
            o = st.next()
            kb.tt(o[:, :], ps2[:, :], a[:, :], ALU.mult, [ps2, a], [o])
            kb.dma("sp", g.actT[c:c + 128, t0:t0 + 512], o[:, :], [o], [])
        segs = [(c * 128, 128, None, False) for c in range(DFF // 128)]
        dense(g, hT, 16, 0, S, Wg, make_blocks(segs, 512), epi, W2=Wu, NB=512)


def phase_ffn2(g, layer):
    kb, nc = g.kb, g.nc
    Wd = g.ffn_down[layer * DFF:(layer + 1) * DFF, :]
    with ExitStack() as es:
        aT = sb(nc, es, "faT", [128, 44, 1024], BF16)
        av = g.actT.rearrange("(c p) s -> p c s", p=128)
        for half in range(2):
            for k0 in range(0, 44, 4):
                kb.dma("sp", aT[:, k0:k0 + 4, :], av[:, k0:k0 + 4, half * 1024:(half + 1) * 1024], [], [aT])
            phase_residual_dense(g, aT, 44, half * 1024, 1024, Wd, g.xres, 256)


def phase_inproj0(g, hT):
    kb, nc = g.kb, g.nc
    with ExitStack() as es:
        s32 = Stager(g, es, "i0s", [128, 512], F32, 3)
        s16 = Stager(g, es, "i0h", [128, 512], BF16, 3)

        def epi(tag, c, width, t0, ps, ps2):
            kind, row = tag
            if kind == "p0":
                o = s32.next()
                kb.cp("act", o[0:width, :], ps[0:width, :], [ps], [o])
                kb.dma("sp", g.P0T[row:row + width, t0:t0 + 512], o[0:width, :], [o], [])
            elif kind == "bq":
                o = s16.next()
                kb.act(o[:, :], ps[:, :], AF.Copy, [ps], [o], scale=128.0 ** -0.5)
                kb.dma("sp", g.bqT[row:row + 128, t0:t0 + 512], o[:, :], [o], [])
            else:
                o = s16.next()
                kb.cp("act", o[:, :], ps[:, :], [ps], [o])
                kb.dma("sp", g.bkT[row:row + 128, t0:t0 + 512], o[:, :], [o], [])

        def tm_epi(c0, w, t0, ps):
            o = s16.next()
            kb.cp("act", o[:, 0:w], ps[:, 0:w], [ps], [o])
            kb.dma("sp", g.bv_tok[t0:t0 + 128, c0 - 6160:c0 - 6160 + w], o[:, 0:w], [o], [])
        segs = []
        for c in range(32):
            segs.append((c * 128, 128, ("p0", c * 128), False))
        segs.append((4096, 16, ("p0", 4096), False))
        for c in range(8):
            segs.append((4112 + c * 128, 128, ("bq", c * 128), False))
        for c in range(8):
            segs.append((5136 + c * 128, 128, ("bk", c * 128), False))
        segs.append((6160, 512, None, True))
        segs.append((6672, 512, None, True))
        dense(g, hT, 16, 0, S, g.w_in_ab, make_blocks(segs, 512), epi, NB=512, tm_epi=tm_epi)


def phase_sb(g):
    kb, nc = g.kb, g.nc
    with ExitStack() as es:
        qTs_ = [sb(nc, es, "sbq%d" % i, [128, S], BF16) for i in range(2)]
        kTs_ = [sb(nc, es, "sbk%d" % i, [128, S], BF16) for i in range(2)]
        vts_ = [sb(nc, es, "sbv%d" % i, [128, 16, 128], BF16) for i in range(2)]
        zl = sb(nc, es, "sbzl", [128, 16, 512], F32)
        spH = sb(nc, es, "sbsh", [128, 16, 512], BF16)
        spL = sb(nc, es, "sbsl", [128, 16, 512], BF16)
        AT = sb(nc, es, "sbat", [128, 16, 512], BF16)
        e1 = Stager(g, es, "sbe1", [128, 512], F32, 2)
        spf = Stager(g, es, "sbsp", [128, 512], F32, 2)
        ee = Stager(g, es, "sbee", [128, 512], F32, 2)
        ost = Stager(g, es, "sbos", [128, 512], BF16, 2)
        tri = sb(nc, es, "sbtri", [128, 128], BF16)
        onb = sb(nc, es, "sbone", [128, 128], BF16)
        mkb = sb(nc, es, "sbmkb", [128, 128], BF16)
        kb.cp("dve", tri[:, :], g.cst[:, C_SBTRI:C_SBTRI + 128], [], [tri])
        kb.cp("dve", onb[:, :], g.cst[:, C_ONES:C_ONES + 128], [], [onb])
        kb.cp("dve", mkb[:, :], g.cst[:, C_SBMSK:C_SBMSK + 128], [], [mkb])
        mkf = g.cst[:, C_SBMSK:C_SBMSK + 128]
        bankR = g.ring.pop()
        bvv = g.bv_tok.rearrange("(n p) c -> p n c", p=128)
        def sb_load(h):
            kb.dma("sp", qTs_[h % 2][:, :], g.bqT[h * 128:(h + 1) * 128, :], [], [qTs_[h % 2]])
            kb.dma("sp", kTs_[h % 2][:, :], g.bkT[h * 128:(h + 1) * 128, :], [], [kTs_[h % 2]])
            kb.dma("sp", vts_[h % 2][:, :, :], bvv[:, :, h * 128:(h + 1) * 128], [], [vts_[h % 2]])
        sb_load(0)
        for h in range(8):
            qT, kT, vt = qTs_[h % 2], kTs_[h % 2], vts_[h % 2]
            if h + 1 < 8:
                sb_load(h + 1)
            for T in range(4):
                nkb = 4 * (T + 1)

                def rng(kbi):
                    off = 0 if kbi < 4 * T else (kbi - 4 * T) * 128
                    return off, 512 - off
                for kbi in range(nkb):
                    off, w = rng(kbi)
                    ps = g.psum()
                    kb.mm(ps[:, off:512], [(kT[:, kbi * 128:(kbi + 1) * 128], qT[:, T * 512 + off:(T + 1) * 512])], [qT, kT], [ps])
                    a = e1.next()
                    kb.act(a[:, off:512], ps[:, off:512], AF.Exp, [ps], [a])
                    s_ = spf.next()
                    kb.act(s_[:, off:512], a[:, off:512], AF.Ln, [a], [s_], bias=g.one[:, 0:1])
                    kb.tt(zl[:, kbi, off:512], ps[:, off:512], s_[:, off:512], ALU.subtract, [ps, s_], [zl])
                    if kbi >= 4 * T:
                        kb.tt(s_[:, off:off + 128], s_[:, off:off + 128], mkf, ALU.mult, [s_], [s_])
                    kb.cp("act", spH[:, kbi, off:512], s_[:, off:512], [s_], [spH])
                    kb.tt(spL[:, kbi, off:512], s_[:, off:512], spH[:, kbi, off:512], ALU.subtract, [s_, spH], [spL])
                for kbi in range(4 * T + 1, nkb):
                    off, w = rng(kbi)
                    kb.op("pool", lambda eng, o=spH[:, kbi, 0:off]: eng.memset(o, 0.0), [], [spH])
                    kb.op("pool", lambda eng, o=spL[:, kbi, 0:off]: eng.memset(o, 0.0), [], [spL])

                def emitB(kbi):
                    off, w = rng(kbi)
                    psB = g.psum()
                    kb.mm(psB[:, off:512], [(tri[:, :], spH[:, kbi, off:512]), (tri[:, :], spL[:, kbi, off:512])], [spH, spL, tri], [psB])
                    return psB
                psB = emitB(nkb - 1)
                for kbi in range(nkb - 1, -1, -1):
                    off, w = rng(kbi)
                    psBn = emitB(kbi - 1) if kbi > 0 else None
                    e_ = ee.next()
                    kb.tt(e_[:, off:512], zl[:, kbi, off:512], psB[:, off:512], ALU.subtract, [zl, psB], [e_])
                    if kbi < nkb - 1:
                        kb.tt(e_[:, off:512], e_[:, off:512], bankR[:, off:512], ALU.subtract, [e_, bankR], [e_])
                    kb.act(AT[:, kbi, off:512], e_[:, off:512], AF.Exp, [e_], [AT])
                    if kbi >= 4 * T:
                        kb.tt(AT[:, kbi, off:off + 128], AT[:, kbi, off:off + 128], mkb[:, :], ALU.mult, [AT, mkb], [AT])
                    if kbi > 0:
                        first = (kbi == nkb - 1)

                        def fnR(eng, kbi=kbi, first=first):
                            eng.matmul(bankR[:, :], onb[:, :], spH[:, kbi, :], start=first, stop=False)
                            return eng.matmul(bankR[:, :], onb[:, :], spL[:, kbi, :], start=False, stop=True)
                        kb.op("pe", fnR, [spH, spL, onb], [bankR])
                    psB = psBn
                ps = g.psum()
                pv = []
                for kbi in range(nkb):
                    off, w = rng(kbi)
                    pv.append((ps[:, off:512], vt[:, kbi, :], AT[:, kbi, off:512]))

                def fn2(eng, pv=pv):
                    ins = None
                    n = len(pv)
                    for i, (o, l, r) in enumerate(pv):
                        ins = eng.matmul(o, l, r, start=(i == 0), stop=(i == n - 1))
                    return ins
                kb.op("pe", fn2, [vt, AT], [ps])
                o = ost.next()
                kb.cp("act", o[:, :], ps[:, :], [ps], [o])
                kb.dma("pool", g.oT[1024 + h * 128:1024 + (h + 1) * 128, T * 512:(T + 1) * 512], o[:, :], [o], [])
        kb.barrier()
        g.ring.append(bankR)


def phase_gdn_pre(g, gtok, btok):
    kb, nc = g.kb, g.nc
    cst = g.cst
    ident = cst[:, C_IDENT:C_IDENT + 128]
    with ExitStack() as es:
        xps = [sb(nc, es, "gxp%d" % i, [128, S + 3], F32) for i in range(2)]
        accs = [sb(nc, es, "gacc%d" % i, [128, S], F32) for i in range(2)]
        ys = [sb(nc, es, "gy%d" % i, [128, S], F32) for i in range(2)]
        ybs = [sb(nc, es, "gyb%d" % i, [128, S], BF16) for i in range(2)]
        sqs = [sb(nc, es, "gsq%d" % i, [128, S], BF16) for i in range(2)]
        rns = [sb(nc, es, "grn%d" % i, [128, S], F32) for i in range(2)]
        stgs = [sb(nc, es, "gstg%d" % i, [64, 32, 128], F32) for i in range(2)]
        for xp in xps:
            kb.op("dve", lambda eng, xp=xp: eng.memset(xp[:, 0:3], 0.0), [], [xp])
        for ci in range(24):
            kind, h = ci // 8, ci % 8
            xp, acc, y, yb, sq, rn, stg = xps[ci % 2], accs[ci % 2], ys[ci % 2], ybs[ci % 2], sqs[ci % 2], rns[ci % 2], stgs[ci % 2]
            kb.dma("sp", xp[:, 3:3 + S], g.P0T[ci * 128:(ci + 1) * 128, :], [], [xp])
            kb.ts(acc[:, :], xp[:, 0:S], g.convw[:, ci * 4:ci * 4 + 1], None, ALU.mult, None, [xp], [acc])
            for i in range(1, 4):
                kb.stt(acc[:, :], xp[:, i:i + S], g.convw[:, ci * 4 + i:ci * 4 + i + 1], acc[:, :], ALU.mult, ALU.add, [xp, acc], [acc])
            kb.act(y[:, :], acc[:, :], AF.Silu, [acc], [y])
            if kind < 2:
                kb.act(sq[:, :], y[:, :], AF.Square, [y], [sq])
                for tt in range(4):
                    ps = g.psum()
                    kb.mm(ps[:, :], [(g.onesb[:, :], sq[:, tt * 512:(tt + 1) * 512])], [sq], [ps])
                    kb.act(rn[:, tt * 512:(tt + 1) * 512], ps[:, :], AF.Ln, [ps], [rn], bias=g.eps[:, 0:1])
                kb.act(rn[:, :], rn[:, :], AF.Exp, [rn], [rn], scale=-0.5)
                sc = (128.0 ** -0.5) if kind == 0 else 1.0
                if kind == 0:
                    kb.stt(yb[:, :], y[:, :], sc, rn[:, :], ALU.mult, ALU.mult, [y, rn], [yb])
                else:
                    kb.stt(y[:, :], y[:, :], sc, rn[:, :], ALU.mult, ALU.mult, [y, rn], [y])
                    kb.cp("act", yb[:, :], y[:, :], [y], [yb])
                dst = g.gqT if kind == 0 else g.gkT
                kb.dma("pool", dst[h * 128:(h + 1) * 128, :], yb[:, :], [yb], [])
            if kind >= 1:
                for n4 in range(8):
                    ps = g.psum()
                    kb.tr_multi([(ps[0:64, j * 128:(j + 1) * 128], y[:, (n4 * 4 + j) * 64:(n4 * 4 + j + 1) * 64]) for j in range(4)],
                                ident, [y], [ps])
                    kb.cp("act", stg[:, n4 * 4:(n4 + 1) * 4, :], ps[0:64, :].rearrange("p (a b) -> p a b", a=4), [ps], [stg])
                dst = g.gk_tok if kind == 1 else g.gv_tok
                kb.dma("pool", dst.rearrange("(n j) c -> j n c", j=64)[:, :, h * 128:(h + 1) * 128], stg[:, :, :], [stg], [])
        al = sb(nc, es, "gal", [8, S], F32)
        be = sb(nc, es, "gbe", [8, S], F32)
        small = sb(nc, es, "gsm", [8, 4], F32)
        kb.dma("sp", al[:, :], g.P0T[4096:4104, :], [], [al])
        kb.dma("sp", be[:, :], g.P0T[4104:4112, :], [], [be])
        kb.act(small[:, 0:1], g.alog[:, 0:1], AF.Exp, [], [small])
        kb.ts(small[:, 1:2], small[:, 0:1], -1.0, None, ALU.mult, None, [small], [small])
        kb.act(al[:, :], al[:, :], AF.Exp, [al], [al], bias=g.dtb[:, 0:1])
        kb.act(al[:, :], al[:, :], AF.Ln, [al], [al], bias=g.one[0:8, 0:1])
        kb.ts(al[:, :], al[:, :], small[:, 1:2], None, ALU.mult, None, [al, small], [al])
        kb.act(be[:, :], be[:, :], AF.Sigmoid, [be], [be])
        for src, dstt in ((al, gtok), (be, btok)):
            ps = g.psum()
            kb.tr_multi([(ps[0:64, n * 8:(n + 1) * 8], src[0:8, n * 64:(n + 1) * 64]) for n in range(32)],
                        cst[0:8, C_IDENT:C_IDENT + 8], [src], [ps])
            kb.cp("act", dstt[:, :, :], ps[0:64, 0:256].rearrange("p (a b) -> p a b", a=32), [ps], [dstt])
        kb.barrier()


def phase_gdn_main(g, gtok, btok):
    kb, nc = g.kb, g.nc
    cst = g.cst
    H = 8
    tri8 = cst[0:64, C_TRI8:C_TRI8 + 512]
    id8 = cst[0:64, C_ID8:C_ID8 + 512]
    ms8 = cst[0:64, C_MS8:C_MS8 + 512]
    U = cst[0:64, C_U:C_U + 64]
    triI = cst[0:64, C_TRI8:C_TRI8 + 64]
    id64 = cst[0:64, C_IDENT:C_IDENT + 64]
    ones = g.ones
    with ExitStack() as es:
        def T64(name, w=512):
            return sb(nc, es, name, [64, w], F32)
        qTs = [sb(nc, es, "mq%d" % i, [128, 8, 256], BF16) for i in range(2)]
        kTs = [sb(nc, es, "mk%d" % i, [128, 8, 256], BF16) for i in range(2)]
        kts = [sb(nc, es, "mkt%d" % i, [64, 1024], F32) for i in range(2)]
        vts = [sb(nc, es, "mvt%d" % i, [64, 1024], F32) for i in range(2)]
        G2, B2, decT, M1, M2 = T64("mG2"), T64("mB2"), T64("mdec"), T64("mM1"), T64("mM2")
        AT = sb(nc, es, "mAT", [64, 512], BF16)
        Qb = [sb(nc, es, "mQ%d" % i, [64, 512], BF16) for i in range(2)]
        Pb = [sb(nc, es, "mP%d" % i, [64, 512], BF16) for i in range(2)]
        Rb = [T64("mR0"), T64("mR1")]
        Rbb = [sb(nc, es, "mRb%d" % i, [64, 512], BF16) for i in range(2)]
        Q0f = T64("mQ0f")
        egbc = sb(nc, es, "megbc", [128, 512], F32)
        esm = T64("mesm", 16)
        bg = T64("mbg", 8)
        u = T64("mu", 1024)
        vb, kbg, kdec, vnew = [sb(nc, es, nm, [64, 1024], BF16) for nm in ("mvb", "mkbg", "mkdec", "mvn")]
        qg = sb(nc, es, "mqg", [128, 512], BF16)
        wT = sb(nc, es, "mwT", [128, 512], BF16)
        St = sb(nc, es, "mS", [128, 1024], F32)
        Stb = sb(nc, es, "mSb", [128, 1024], BF16)
        oraw = sb(nc, es, "mor", [128, 8, 256], F32)
        osq = sb(nc, es, "mosq", [128, 8, 256], BF16)
        gate = sb(nc, es, "mgate", [128, 8, 256], F32)
        rsd = sb(nc, es, "mrsd", [128, 8, 256], F32)
        oout = sb(nc, es, "moo", [128, 8, 256], BF16)
        kb.op("dve", lambda eng: eng.memset(St[:, :], 0.0), [], [St])
        gqv = g.gqT.rearrange("(h p) s -> p h s", p=128)
        gkv = g.gkT.rearrange("(h p) s -> p h s", p=128)
        ktv = g.gk_tok.rearrange("(n j) c -> j n c", j=64)
        vtv = g.gv_tok.rearrange("(n j) c -> j n c", j=64)
        gatev = g.P0T[3072:4096, :].rearrange("(h p) s -> p h s", p=128)
        oTv = g.oT[0:1024, :].rearrange("(h p) s -> p h s", p=128)

        def v3(t, w=64):
            return t[:, :].rearrange("p (h i) -> p h i", h=H)

        def load_group(gi):
            b = gi % 2
            kb.dma("sp", qTs[b][:, :, :], gqv[:, :, gi * 256:(gi + 1) * 256], [], [qTs[b]])
            kb.dma("sp", kTs[b][:, :, :], gkv[:, :, gi * 256:(gi + 1) * 256], [], [kTs[b]])
        load_group(0)
        for n in range(32):
            gi, nl = n // 4, n % 4
            b = gi % 2
            if nl == 0 and gi + 1 < 8:
                load_group(gi + 1)
            if nl == 0:
                kb.dma("sp", gate[:, :, :], gatev[:, :, gi * 256:(gi + 1) * 256], [], [gate])
            qT, kT, kt, vt = qTs[b], kTs[b], kts[n % 2], vts[n % 2]
            kb.dma("sp", kt[:, :], ktv[:, n, :], [], [kt])
            kb.dma("sp", vt[:, :], vtv[:, n, :], [], [vt])
            cs = slice(nl * 64, (nl + 1) * 64)
            gcol = gtok[:, n, :]
            bcol = btok[:, n, :]
            kb.tt(v3(G2), tri8.rearrange("p (h i) -> p h i", h=H), gcol.unsqueeze(2).to_broadcast([64, 8, 64]), ALU.mult, [gtok], [G2])
            kb.tt(v3(B2), id8.rearrange("p (h i) -> p h i", h=H), bcol.unsqueeze(2).to_broadcast([64, 8, 64]), ALU.mult, [btok], [B2])
            psD, psB, psG, psS = g.psum(), g.psum(), g.psum(), g.psum()
            kb.mm(psD[0:64, :], [(U, G2[:, :])], [G2], [psD])
            kb.mm(psB[0:64, :], [(ones[0:64, 0:64], B2[:, :])], [B2], [psB])
            kb.mm(psG[:, :], [(ones[0:64, :], G2[:, :])], [G2], [psG])
            kb.mm_multi([(psS[0:64, 0:8], [(triI, gcol)]), (psS[0:64, 8:16], [(U, gcol)])], [gtok], [psS])
            kb.act(decT[:, :], psD[0:64, :], AF.Exp, [psD], [decT])
            kb.act(egbc[:, :], psG[:, :], AF.Exp, [psG], [egbc])
            kb.act(esm[:, :], psS[0:64, 0:16], AF.Exp, [psS], [esm])
            kb.tt(M1[:, :], decT[:, :], tri8, ALU.mult, [decT], [M1])
            kb.tt(M2[:, :], decT[:, :], ms8, ALU.mult, [decT], [M2])
            kb.tt(M2[:, :], psB[0:64, :], M2[:, :], ALU.mult, [psB, M2], [M2])
            pskk, psqk = g.psum(), g.psum()
            kb.mm_multi([(pskk[0:64, h * 64:(h + 1) * 64], [(kT[:, h, cs], kT[:, h, cs])]) for h in range(H)], [kT], [pskk])
            kb.mm_multi([(psqk[0:64, h * 64:(h + 1) * 64], [(kT[:, h, cs], qT[:, h, cs])]) for h in range(H)], [kT, qT], [psqk])
            kb.tt(AT[:, :], psqk[0:64, :], M1[:, :], ALU.mult, [psqk, M1], [AT])
            Q, P_, R, Rh = Qb[0], Pb[0], Rb[0], Rbb[0]
            kb.stt(Q0f[:, :], pskk[0:64, :], -1.0, M2[:, :], ALU.mult, ALU.mult, [pskk, M2], [Q0f])
            psT = g.psum()
            kb.tr_multi([(psT[0:64, h * 64:(h + 1) * 64], Q0f[:, h * 64:(h + 1) * 64]) for h in range(H)], id64, [Q0f], [psT])
            kb.cp("act", P_[:, :], psT[0:64, :], [psT], [P_])
            kb.cp("act", Q[:, :], Q0f[:, :], [Q0f], [Q])
            kb.tt(R[:, :], Q0f[:, :], id8, ALU.add, [Q0f], [R])
            kb.cp("act", Rh[:, :], R[:, :], [R], [Rh])
            for lv in range(1, 6):
                Qn, Pn, Rn, Rhn = Qb[lv % 2], Pb[lv % 2], Rb[lv % 2], Rbb[lv % 2]
                psP = g.psum()
                kb.mm_multi([(psP[0:64, h * 64:(h + 1) * 64], [(Q[:, h * 64:(h + 1) * 64], P_[:, h * 64:(h + 1) * 64])]) for h in range(H)], [Q, P_], [psP])
                if lv < 5:
                    psQ = g.psum()
                    kb.mm_multi([(psQ[0:64, h * 64:(h + 1) * 64], [(P_[:, h * 64:(h + 1) * 64], Q[:, h * 64:(h + 1) * 64])]) for h in range(H)], [Q, P_], [psQ])
                kb.cp("act", Pn[:, :], psP[0:64, :], [psP], [Pn])
                if lv < 5:
                    kb.cp("dve", Qn[:, :], psQ[0:64, :], [psQ], [Qn])
                psR = g.psum()
                kb.mm_multi([(psR[0:64, h * 64:(h + 1) * 64], [(Pn[:, h * 64:(h + 1) * 64], Rh[:, h * 64:(h + 1) * 64])]) for h in range(H)], [Pn, Rh], [psR])
                kb.tt(Rn[:, :], psR[0:64, :], R[:, :], ALU.add, [psR, R], [Rn])
                kb.cp("act", Rhn[:, :], Rn[:, :], [Rn], [Rhn])
                Q, P_, R, Rh = Qn, Pn, Rn, Rhn
            ktc = kt[:, :].rearrange("p (h d) -> p h d", h=H)
            vtc = vt[:, :].rearrange("p (h d) -> p h d", h=H)
            kb.tt(vb[:, :].rearrange("p (h d) -> p h d", h=H), vtc, bcol.unsqueeze(2).to_broadcast([64, 8, 128]), ALU.mult, [vt, btok], [vb])
            kb.tt(bg[:, :], bcol, esm[:, 0:8], ALU.mult, [btok, esm], [bg])
            kb.tt(kbg[:, :].rearrange("p (h d) -> p h d", h=H), ktc, bg[:, :].unsqueeze(2).to_broadcast([64, 8, 128]), ALU.mult, [kt, bg], [kbg])
            kb.tt(kdec[:, :].rearrange("p (h d) -> p h d", h=H), ktc, esm[:, 8:16].unsqueeze(2).to_broadcast([64, 8, 128]), ALU.mult, [kt, esm], [kdec])
            kb.tt(qg[:, :].rearrange("p (h i) -> p h i", h=H), qT[:, :, cs], egbc[:, :].rearrange("p (h i) -> p h i", h=H), ALU.mult, [qT, egbc], [qg])
            psu0, psu1, psw = g.psum(), g.psum(), g.psum()
            kb.mm_multi([((psu0 if h < 4 else psu1)[0:64, (h % 4) * 128:(h % 4 + 1) * 128], [(Rh[:, h * 64:(h + 1) * 64], vb[:, h * 128:(h + 1) * 128])]) for h in range(H)],
                        [Rh, vb], [psu0, psu1])
            kb.mm_multi([(psw[:, h * 64:(h + 1) * 64], [(kbg[:, h * 128:(h + 1) * 128], Rh[:, h * 64:(h + 1) * 64])]) for h in range(H)], [kbg, Rh], [psw])
            kb.cp("act", u[:, 0:512], psu0[0:64, :], [psu0], [u])
            kb.cp("act", u[:, 512:1024], psu1[0:64, :], [psu1], [u])
            kb.cp("act", wT[:, :], psw[:, :], [psw], [wT])
            kb.cp("act", Stb[:, :], St[:, :], [St], [Stb])
            pws0, pws1 = g.psum(), g.psum()
            kb.mm_multi([((pws0 if h < 4 else pws1)[0:64, (h % 4) * 128:(h % 4 + 1) * 128], [(wT[:, h * 64:(h + 1) * 64], Stb[:, h * 128:(h + 1) * 128])]) for h in range(H)],
                        [wT, Stb], [pws0, pws1])
            kb.tt(vnew[:, 0:512], u[:, 0:512], pws0[0:64, :], ALU.subtract, [u, pws0], [vnew])
            kb.tt(vnew[:, 512:1024], u[:, 512:1024], pws1[0:64, :], ALU.subtract, [u, pws1], [vnew])
            pso = g.psum()
            kb.mm_multi([(pso[:, h * 64:(h + 1) * 64], [(Stb[:, h * 128:(h + 1) * 128], qg[:, h * 64:(h + 1) * 64]),
                                                         (vnew[:, h * 128:(h + 1) * 128], AT[:, h * 64:(h + 1) * 64])]) for h in range(H)],
                        [Stb, qg, vnew, AT], [pso])
            kb.cp("act", oraw[:, :, cs], pso[:, :].rearrange("p (h i) -> p h i", h=H), [pso], [oraw])
            psu0, psu1 = g.psum(), g.psum()
            kb.mm_multi([((psu0 if h < 4 else psu1)[:, (h % 4) * 128:(h % 4 + 1) * 128], [(kdec[:, h * 128:(h + 1) * 128], vnew[:, h * 128:(h + 1) * 128])]) for h in range(H)],
                        [kdec, vnew], [psu0, psu1])
            kb.tt(St[:, :].rearrange("p (h d) -> p h d", h=H), St[:, :].rearrange("p (h d) -> p h d", h=H),
                  egbc[:, :].rearrange("p (h i) -> p h i", h=H)[:, :, 63:64].to_broadcast([128, 8, 128]), ALU.mult, [St, egbc], [St])
            kb.tt(St[:, 0:512], St[:, 0:512], psu0[:, :], ALU.add, [St, psu0], [St])
            kb.tt(St[:, 512:1024], St[:, 512:1024], psu1[:, :], ALU.add, [St, psu1], [St])
            if nl == 3:
                kb.act(osq[:, :, :], oraw[:, :, :], AF.Square, [oraw], [osq])
                for q4 in range(4):
                    ps = g.psum()
                    kb.mm(ps[:, :], [(g.onesb[:, :], osq[:, 2 * q4:2 * q4 + 2, :])], [osq], [ps])
                    kb.act(rsd[:, 2 * q4:2 * q4 + 2, :], ps[:, :].rearrange("p (a b) -> p a b", a=2), AF.Ln, [ps], [rsd], bias=g.eps[:, 0:1], scale=1.0 / 128)
                kb.act(rsd[:, :, :], rsd[:, :, :], AF.Exp, [rsd], [rsd], scale=-0.5)
                kb.act(gate[:, :, :], gate[:, :, :], AF.Silu, [gate], [gate])
                kb.stt(oraw[:, :, :], oraw[:, :, :], g.gnorm[:, 0:1], rsd[:, :, :], ALU.mult, ALU.mult, [oraw, rsd], [oraw])
                kb.tt(oout[:, :, :], oraw[:, :, :], gate[:, :, :], ALU.mult, [oraw, gate], [oout])
                kb.dma("pool", oTv[:, :, gi * 256:(gi + 1) * 256], oout[:, :, :], [oout], [])
        kb.barrier()


PI = 3.14159265358979
PI_LO = 3.1415925


def build_rope_tables(g, es, pos):
    kb, nc = g.kb, g.nc
    tabs = {}
    for nm in ("q", "i"):
        for which in ("sin", "cos"):
            tabs[nm + which] = sb(nc, es, "rt" + nm + which, [128, S], F32)
    with ExitStack() as tes:
        posi = sb(nc, tes, "rpi", [128, S], I32)
        posf = sb(nc, tes, "rpf", [128, S], F32)
        a = sb(nc, tes, "rpa", [128, S], F32)
        ni = sb(nc, tes, "rpn", [128, S], I32)
        nf = sb(nc, tes, "rpnf", [128, S], F32)
        m = sb(nc, tes, "rpm", [128, S], F32)
        kb.dma("sp", posi[:, :], pos.partition_broadcast(128)[:, 0, :], [], [posi])
        kb.cp("dve", posf[:, :], posi[:, :], [posi], [posf])
        for nm, fcol in (("q", C_FQ), ("i", C_FI)):
            for which, shift in (("sin", 0.0), ("cos", PI / 2)):
                out = tabs[nm + which]
                kb.ts(a[:, :], posf[:, :], g.cst[:, fcol:fcol + 1], shift, ALU.mult, ALU.add, [posf], [a])
                kb.ts(ni[:, :], a[:, :], 1.0 / (2 * PI), None, ALU.mult, None, [a], [ni])
                kb.cp("dve", nf[:, :], ni[:, :], [ni], [nf])
                kb.stt(a[:, :], nf[:, :], -2 * PI, a[:, :], ALU.mult, ALU.add, [nf, a], [a])
                kb.ts(m[:, :], a[:, :], PI, None, ALU.is_gt, None, [a], [m])
                kb.stt(a[:, :], m[:, :], -2 * PI, a[:, :], ALU.mult, ALU.add, [m, a], [a])
                kb.ts(m[:, :], a[:, :], -PI, None, ALU.is_lt, None, [a], [m])
                kb.stt(a[:, :], m[:, :], 2 * PI, a[:, :], ALU.mult, ALU.add, [m, a], [a])
                kb.ts(a[:, :], a[:, :], PI_LO, -PI_LO, ALU.min, ALU.max, [a], [a])
                kb.act(out[:, :], a[:, :], AF.Sin, [a], [out])
        kb.barrier()
    return tabs


def phase_inproj1(g, hT, tabs):
    kb, nc = g.kb, g.nc
    with ExitStack() as es:
        s32 = Stager(g, es, "i1s", [128, 512], F32, 3)
        s16 = Stager(g, es, "i1h", [128, 512], BF16, 3)
        xs_ = Stager(g, es, "i1x", [128, 512], F32, 3)
        xb_ = Stager(g, es, "i1xb", [128, 512], BF16, 3)
        t1_ = Stager(g, es, "i1a", [128, 512], F32, 2)
        t2_ = Stager(g, es, "i1b", [128, 512], F32, 2)

        def epi(tag, c, width, t0, ps, ps2):
            kind, row = tag
            if kind == "wi":
                o = s32.next()
                kb.act(o[0:width, :], ps[0:width, :], AF.Copy, [ps], [o], scale=(16 ** -0.5) * (64 ** -0.5))
                kb.dma("sp", g.cwiT[0:width, t0:t0 + 512], o[0:width, :], [o], [])
                return
            isq = kind in ("q", "k")
            R = (g.rqb if isq else g.rib)[0:width, 0:width]
            cos = tabs["qcos" if isq else "icos"]
            sin = tabs["qsin" if isq else "isin"]
            sc = 128.0 ** -0.5 if kind == "q" else 1.0
            x = xs_.next()
            kb.cp("act", x[0:width, :], ps[0:width, :], [ps], [x])
            xb = xb_.next()
            kb.cp("act", xb[0:width, :], ps[0:width, :], [ps], [xb])

            def late():
                pr = g.psum()
                kb.mm(pr[0:width, :], [(R, xb[0:width, :])], [xb], [pr])
                t1 = t1_.next()
                t2 = t2_.next()
                kb.stt(t1[0:width, :], x[0:width, :], sc, cos[0:width, t0:t0 + 512], ALU.mult, ALU.mult, [x, cos], [t1])
                kb.stt(t2[0:width, :], pr[0:width, :], sc, sin[0:width, t0:t0 + 512], ALU.mult, ALU.mult, [pr, sin], [t2])
                if kind in ("q", "k"):
                    o = s16.next()
                    dst = g.cqT if kind == "q" else g.ckT
                else:
                    o = s32.next()
                    dst = g.cqiT if kind == "qi" else g.ckiT
                kb.tt(o[0:width, :], t1[0:width, :], t2[0:width, :], ALU.add, [t1, t2], [o])
                kb.dma("sp", dst[row:row + width, t0:t0 + 512], o[0:width, :], [o], [])
            return late

        def tm_epi(c0, w, t0, ps):
            o = s16.next()
            kb.cp("act", o[:, 0:w], ps[:, 0:w], [ps], [o])
            kb.dma("sp", g.cv_tok[t0:t0 + 128, 0:w], o[:, 0:w], [o], [])
        segs = []
        for c in range(16):
            segs.append((c * 128, 128, ("q", c * 128), False))
        for c in range(4):
            segs.append((2048 + c * 128, 128, ("k", c * 128), False))
        segs.append((2560, 512, None, True))
        for c in range(8):
            segs.append((3072 + c * 128, 128, ("qi", c * 128), False))
        segs.append((4096, 64, ("ki", 0), False))
        segs.append((4160, 16, ("wi", 0), False))
        dense(g, hT, 16, 0, S, g.w_in_c, make_blocks(segs, 512), epi, NB=512, tm_epi=tm_epi)


def phase_dsa(g):
    kb, nc = g.kb, g.nc
    cst = g.cst
    ident = cst[:, C_IDENT:C_IDENT + 128]
    CB = cst[:, C_CB:C_CB + 128]
    with ExitStack() as es:
        ki2 = sb(nc, es, "dki2", [128, S], F32)
        kiH2 = sb(nc, es, "dkiH", [128, S], BF16)
        kiL2 = sb(nc, es, "dkiL", [128, S], BF16)
        kT = sb(nc, es, "dk", [128, 4, S], BF16)
        vt = sb(nc, es, "dv", [128, 16, 512], BF16)
        wiT = sb(nc, es, "dwiT", [16, S], F32)
        witok = sb(nc, es, "dwit", [128, 16, 16], F32)
        wabs = sb(nc, es, "dwa", [128, 16, 16], F32)
        wsgn = sb(nc, es, "dws", [128, 16, 16], F32)
        qi2 = sb(nc, es, "dqi2", [128, 16, 128], F32)
        qib = [sb(nc, es, "dqi%d" % i, [128, 16, 128], BF16) for i in range(2)]
        qb = [sb(nc, es, "dq%d" % i, [128, 16, 128], BF16) for i in range(2)]
        score = sb(nc, es, "dsc", [128, S], F32)
        score1 = sb(nc, es, "dsc1", [128, S], F32)
        mbb1 = sb(nc, es, "dmbb1", [128, S], BF16)
        work = sb(nc, es, "dwk", [128, S], F32)
        mb = sb(nc, es, "dmb", [128, S], F32)
        mbb = sb(nc, es, "dmbb", [128, S], BF16)
        identb = sb(nc, es, "didb", [128, 128], BF16)
        Pms = [sb(nc, es, "dP%d" % i, [128, S], BF16) for i in range(2)]
        PTs = [sb(nc, es, "dPT%d" % i, [128, 16, 128], BF16) for i in range(2)]
        sts = [sb(nc, es, "dst%d" % i, [128, 16], F32) for i in range(2)]
        oblk = sb(nc, es, "dob", [128, 16, 128], F32)
        oTb = sb(nc, es, "doT", [128, 16, 128], BF16)
        m8 = sb(nc, es, "dm8", [128, 8], F32)
        rl_ = Stager(g, es, "drl", [128, 512], F32, 3)
        g.tsi = 0
        kb.cp("dve", identb[:, :], cst[:, C_IDENT:C_IDENT + 128], [], [identb])
        kb.dma("sp", ki2[0:64, :], g.ckiT[:, :], [], [ki2])
        kb.dma("sp", ki2[64:128, :], g.ckiT[:, :], [], [ki2])
        kb.cp("act", kiH2[:, :], ki2[:, :], [ki2], [kiH2])
        kb.tt(kiL2[:, :], ki2[:, :], kiH2[:, :], ALU.subtract, [ki2, kiH2], [kiL2])
        kb.dma("sp", kT[:, :, :], g.ckT.rearrange("(g p) s -> p g s", p=128), [], [kT])
        kb.dma("sp", vt[:, :, :], g.cv_tok.rearrange("(n p) c -> p n c", p=128), [], [vt])
        kb.dma("sp", wiT[:, :], g.cwiT[:, :], [], [wiT])
        ps = g.psum()
        kb.tr_multi([(ps[:, n * 16:(n + 1) * 16], wiT[0:16, n * 128:(n + 1) * 128]) for n in range(16)],
                    cst[0:16, C_IDENT:C_IDENT + 16], [wiT], [ps])
        kb.cp("act", witok[:, :, :], ps[:, 0:256].rearrange("p (a b) -> p a b", a=16), [ps], [witok])
        kb.act(wabs[:, :, :], witok[:, :, :], AF.Abs, [witok], [wabs])
        kb.act(wsgn[:, :, :], witok[:, :, :], AF.Sign, [witok], [wsgn])
        ksq = sb(nc, es, "dksq", [128, 4, S], BF16)
        kpart = sb(nc, es, "dkpart", [128, 16], F32)
        kmx = sb(nc, es, "dkmx", [128, 4], F32)
        qsqs = [sb(nc, es, "dqsq%d" % i, [128, 16, 128], BF16) for i in range(2)]
        negBs = [sb(nc, es, "dnegB%d" % i, [128, 16], F32) for i in range(2)]
        kb.act(ksq[:, :, :], kT[:, :, :], AF.Square, [kT], [ksq])
        for gk in range(4):
            for t4 in range(4):
                ps = g.psum()
                kb.mm(ps[:, :], [(g.onesb[:, :], ksq[:, gk, t4 * 512:(t4 + 1) * 512])], [ksq], [ps])
                kb.op("dve", lambda eng, ps=ps, o=kpart[:, gk * 4 + t4:gk * 4 + t4 + 1]: eng.tensor_reduce(out=o, in_=ps[:, :], axis=mybir.AxisListType.X, op=ALU.max), [ps], [kpart])
        kb.op("dve", lambda eng: eng.tensor_reduce(out=kmx[:, :], in_=kpart[:, :].rearrange("p (a b) -> p a b", a=4), axis=mybir.AxisListType.X, op=ALU.max), [kpart], [kmx])
        qiv = g.cqiT.rearrange("(h e) s -> e h s", e=64)
        qv = g.cqT.rearrange("(h p) s -> p h s", p=128)
        oTv = g.oT.rearrange("(h p) s -> p h s", p=128)
        scores = [score, score1]
        mbbs = [mbb, mbb1]

        def tiles_of(b):
            ncols = (b + 1) * 128
            return ncols, [(c0, min(512, ncols - c0)) for c0 in range(0, ncols, 512)]

        def indexer_units(b):
            qi, sc = qib[b % 2], scores[b % 2]
            ncols, tiles = tiles_of(b)
            units = []

            def load():
                kb.dma("sp", qi2[0:64, :, :], qiv[:, :, b * 128:(b + 1) * 128], [], [qi2])
                kb.dma("sp", qi2[64:128, :, :], qiv[:, :, b * 128:(b + 1) * 128], [], [qi2])
                kb.cp("act", qi[:, :, :], qi2[:, :, :], [qi2], [qi])
                kb.tt(qi[64:128, :, :], qi2[64:128, :, :], qi[64:128, :, :], ALU.subtract, [qi2, qi], [qi])
            units.append(load)
            for h in range(16):
                def u(h=h):
                    for (c0, w) in tiles:
                        ps = g.psum()
                        kb.mm(ps[:, 0:w], [(qi[:, h, :], kiH2[:, c0:c0 + w]), (qi[:, h, :], kiL2[:, c0:c0 + w])], [qi, kiH2, kiL2], [ps])
                        rl = rl_.next()
                        kb.act(rl[:, 0:w], ps[:, 0:w], AF.Relu, [ps, wabs], [rl], scale=wabs[:, b, h:h + 1])
                        if h == 0:
                            kb.ts(sc[:, c0:c0 + w], rl[:, 0:w], wsgn[:, b, 0:1], None, ALU.mult, None, [rl, wsgn], [sc])
                        else:
                            kb.stt(sc[:, c0:c0 + w], rl[:, 0:w], wsgn[:, b, h:h + 1], sc[:, c0:c0 + w], ALU.mult, ALU.add, [rl, wsgn, sc], [sc])
                units.append(u)

            def fin():
                kb.tt(sc[:, b * 128:(b + 1) * 128], sc[:, b * 128:(b + 1) * 128], CB, ALU.add, [sc], [sc])
            units.append(fin)
            return units

        def topk_units(b):
            sc, mbb_ = scores[b % 2], mbbs[b % 2]
            ncols, tiles = tiles_of(b)
            units = []
            if b >= 2:
                for r2 in range(16):
                    def u(r2=r2):
                        for r in (2 * r2, 2 * r2 + 1):
                            cur = sc if r == 0 else work
                            kb.op("dve", lambda eng, c=cur, n_=ncols: eng.max(out=m8[:, :], in_=c[:, 0:n_]), [cur], [m8])
                            if r < 31:
                                kb.op("dve", lambda eng, c=cur, n_=ncols: eng.match_replace(out=work[:, 0:n_], in_to_replace=m8[:, :], in_values=c[:, 0:n_], imm_value=NEG),
                                      [cur, m8], [work])
                    units.append(u)

            def fin():
                if b >= 2:
                    kb.ts(mb[:, 0:ncols], sc[:, 0:ncols], m8[:, 7:8], None, ALU.is_ge, None, [sc, m8], [mb])
                else:
                    kb.ts(mb[:, 0:ncols], sc[:, 0:ncols], -1.0e29, None, ALU.is_gt, None, [sc], [mb])
                kb.ts(mbb_[:, 0:ncols], mb[:, 0:ncols], -1.0, 1.0e30, ALU.add, ALU.mult, [mb], [mbb_])
            units.append(fin)
            return units

        def attn_block(b, hook):
            q, mbb_ = qb[b % 2], mbbs[b % 2]
            ncols, tiles = tiles_of(b)
            nt = len(tiles)
            kb.dma("sp", q[:, :, :], qv[:, :, b * 128:(b + 1) * 128], [], [q])
            qsq, negB = qsqs[b % 2], negBs[b % 2]
            kb.act(qsq[:, :, :], q[:, :, :], AF.Square, [q], [qsq])
            psn = g.psum()
            kb.mm_multi([(psn[:, 2 * h:2 * h + 2], [(qsq[:, h, :], g.onesb[:, 0:2])]) for h in range(16)], [qsq], [psn])
            kb.tt(negB[:, :].rearrange("p (a j) -> p a j", a=4),
                  psn[:, 0:32].rearrange("p (a j two) -> p a j two", a=4, two=2)[:, :, :, 0],
                  kmx[:, :].unsqueeze(2).to_broadcast([128, 4, 4]), ALU.mult, [psn, kmx], [negB])
            kb.act(negB[:, :], negB[:, :], AF.Sqrt, [negB], [negB])
            kb.ts(negB[:, :], negB[:, :], -1.0, None, ALU.mult, None, [negB], [negB])

            def stageA(h):
                gq = h // 4
                Pm, st = Pms[h % 2], sts[h % 2]
                pz = []
                for ti, (c0, w) in enumerate(tiles):
                    ps = g.psum()
                    kb.mm(ps[:, 0:w], [(q[:, h, :], kT[:, gq, c0:c0 + w]), (identb[:, :], mbb_[:, c0:c0 + w])], [q, kT, mbb_, identb], [ps])
                    pz.append(ps)
                for ti, (c0, w) in enumerate(tiles):
                    kb.act(Pm[:, c0:c0 + w], pz[ti][:, 0:w], AF.Exp, [pz[ti], negB], [Pm, st], bias=negB[:, h:h + 1], accum=st[:, 8 + ti:9 + ti])
                if nt > 1:
                    kb.op("dve", lambda eng, o=st[:, 2:3], i=st[:, 8:8 + nt]: eng.tensor_reduce(out=o, in_=i, axis=mybir.AxisListType.X, op=ALU.add), [st], [st])
                    rsum = st[:, 2:3]
                else:
                    rsum = st[:, 8:9]
                kb.op("dve", lambda eng, o=st[:, 3:4], i=rsum: eng.reciprocal(out=o, in_=i), [st], [st])

            def stageB(h):
                gq = h // 4
                Pm, PT, st = Pms[h % 2], PTs[h % 2], sts[h % 2]
                for k8 in range(0, b + 1, 8):
                    nk = min(8, b + 1 - k8)
                    ps = g.tslots[g.tsi % 2]
                    g.tsi += 1
                    kb.tr_multi([(ps[:, j * 128:(j + 1) * 128], Pm[:, (k8 + j) * 128:(k8 + j + 1) * 128]) for j in range(nk)], identb[:, :], [Pm, identb], [ps])
                    kb.cp("act", PT[:, k8:k8 + nk, :], ps[:, 0:nk * 128].rearrange("p (a b) -> p a b", a=nk), [ps], [PT])
                ps = g.psum()
                kb.mm(ps[:, 0:128], [(PT[:, k2, :], vt[:, k2, gq * 128:(gq + 1) * 128]) for k2 in range(b + 1)], [PT, vt], [ps])
                kb.act(oblk[:, h, :], ps[:, 0:128], AF.Copy, [ps, st], [oblk], scale=st[:, 3:4])
            stageA(0)
            for h in range(16):
                if h + 1 < 16:
                    stageA(h + 1)
                stageB(h)
                hook()
            for h4 in range(4):
                ps = g.psum()
                kb.tr_multi([(ps[:, j * 128:(j + 1) * 128], oblk[:, h4 * 4 + j, :]) for j in range(4)], ident, [oblk], [ps])
                kb.cp("act", oTb[:, h4 * 4:(h4 + 1) * 4, :], ps[:, :].rearrange("p (a b) -> p a b", a=4), [ps], [oTb])
            kb.dma("pool", oTv[:, :, b * 128:(b + 1) * 128], oTb[:, :, :], [oTb], [])

        for u in indexer_units(0):
            u()
        for u in topk_units(0):
            u()
        for u in indexer_units(1):
            u()
        for b in range(16):
            lists = []
            if b + 1 < 16:
                lists.append(topk_units(b + 1))
            if b + 2 < 16:
                lists.append(indexer_units(b + 2))
            pos = [0] * len(lists)
            per = [-(-len(l) // 16) for l in lists]

            def hook():
                for li, l in enumerate(lists):
                    for _ in range(per[li]):
                        if pos[li] < len(l):
                            l[pos[li]]()
                            pos[li] += 1
            attn_block(b, hook)
            for li, l in enumerate(lists):
                while pos[li] < len(l):
                    l[pos[li]]()
                    pos[li] += 1
        kb.barrier()


def phase_final_norm(g, xsrc, gain_ap, dst):
    kb, nc = g.kb, g.nc
    with ExitStack() as es:
        xs = [sb(nc, es, "fx%d" % i, [128, 16, 512], F32) for i in range(2)]
        sq = sb(nc, es, "fsq", [128, 16, 512], BF16)
        ob = sb(nc, es, "fob", [128, 16, 512], F32)
        rs = sb(nc, es, "frs", [128, 512], F32)
        xv = xsrc.rearrange("(c p) s -> p c s", p=128)
        dv = dst.rearrange("(c p) s -> p c s", p=128)
        for t in range(4):
            x = xs[t % 2]
            for q4 in range(4):
                kb.dma("sp", x[:, q4 * 4:(q4 + 1) * 4, :], xv[:, q4 * 4:(q4 + 1) * 4, t * 512:(t + 1) * 512], [], [x])
            kb.act(sq[:, :, :], x[:, :, :], AF.Square, [x], [sq])
            ps = g.psum()
            kb.mm(ps[:, :], [(g.onesb[:, :], sq[:, c, :]) for c in range(16)], [sq], [ps])
            kb.act(rs[:, :], ps[:, :], AF.Ln, [ps], [rs], bias=g.eps[:, 0:1], scale=1.0 / D)
            kb.act(rs[:, :], rs[:, :], AF.Exp, [rs], [rs], scale=-0.5)
            for c in range(16):
                kb.stt(ob[:, c, :], x[:, c, :], gain_ap[:, c:c + 1], rs[:, :], ALU.mult, ALU.mult, [x, rs], [ob])
            for q4 in range(4):
                kb.dma("sp", dv[:, q4 * 4:(q4 + 1) * 4, t * 512:(t + 1) * 512], ob[:, q4 * 4:(q4 + 1) * 4, :], [ob], [])
        kb.barrier()


def build_program(stop_after=None, dumps=(), skip_l0=False):
    nc = bass.Bass("TRN2", target_bir_lowering=False)
    g = Ctx()
    g.nc = nc

    def din(name, shape, dt=F32):
        return nc.dram_tensor(name, list(shape), dt, kind="ExternalInput").ap()

    def dscr(name, shape, dt=F32):
        return nc.dram_tensor(name, list(shape), dt).ap()
    xT = din("xT", [D, S])
    pos = din("pos", [1, S], I32)
    nmix = din("nmix", [128, 32])
    nffn = din("nffn", [128, 32])
    fnorm = din("fnorm", [128, 16])
    g.w_in_ab = din("w_in_ab", [D, AB_IN])
    convw = din("convw", [128, 96])
    alog = din("alog", [8, 1])
    dtb = din("dtb", [8, 1])
    gnorm = din("gnorm", [128, 1])
    g.w_out_ab = din("w_out_ab", [D, D])
    g.w_in_c = din("w_in_c", [D, C_IN])
    g.w_out_c = din("w_out_c", [D, D])
    g.ffn_gate = din("ffn_gate", [2 * D, DFF])
    g.ffn_up = din("ffn_up", [2 * D, DFF])
    g.ffn_down = din("ffn_down", [2 * DFF, D])
    consts = din("consts", [128, NCONST])
    outT = nc.dram_tensor("outT", [D, S], F32, kind="ExternalOutput").ap()
    dump_aps = {}
    for (nm, shape, dt) in dumps:
        dump_aps[nm] = nc.dram_tensor("dump_" + nm, list(shape), dt, kind="ExternalOutput").ap()

    def scr(name, shape, dt=F32):
        if name in dump_aps:
            return dump_aps[name]
        return dscr(name, shape, dt)
    g.xres = scr("xres", [D, S])
    g.P0T = scr("P0T", [4112, S])
    g.bqT = scr("bqT", [1024, S], BF16)
    g.bkT = scr("bkT", [1024, S], BF16)
    g.bv_tok = scr("bv_tok", [S, 1024], BF16)
    g.gqT = scr("gqT", [1024, S], BF16)
    g.gkT = scr("gkT", [1024, S], BF16)
    g.gk_tok = scr("gk_tok", [S, 1024])
    g.gv_tok = scr("gv_tok", [S, 1024])
    g.oT = scr("oT", [D, S], BF16)
    g.actT = scr("actT", [DFF, S], BF16)
    g.cqT = scr("cqT", [2048, S], BF16)
    g.ckT = scr("ckT", [512, S], BF16)
    g.cv_tok = scr("cv_tok", [S, 512], BF16)
    g.cqiT = scr("cqiT", [1024, S])
    g.ckiT = scr("ckiT", [64, S])
    g.cwiT = scr("cwiT", [16, S])

    with ExitStack() as es:
        kb = KB(nc, es)
        g.kb = kb
        cst = sb(nc, es, "cst", [128, NCONST], F32)
        g.cst = cst.t
        g.ones = cst.t[:, C_ONES:C_ONES + 128]
        small = sb(nc, es, "csm", [128, 4], F32)
        g.eps = small.t[:, 0:1]
        g.one = small.t[:, 1:2]
        gains = sb(nc, es, "gains", [128, 80], F32)
        cw = sb(nc, es, "convw", [128, 96], F32)
        g.convw = cw.t
        sm8 = sb(nc, es, "sm8", [8, 2], F32)
        g.alog = sm8.t[:, 0:1]
        g.dtb = sm8.t[:, 1:2]
        gn = sb(nc, es, "gn", [128, 1], F32)
        g.gnorm = gn.t
        ps2 = [es.enter_context(nc.psum_tensor("ps%d" % i, [128, 1024], F32)) for i in range(3)]
        psb = es.enter_context(nc.psum_tensor("psb", [128, 2048], BF16))
        banks = []
        for i in range(6):
            t = Tl(ps2[i // 2])
            t.t = ps2[i // 2][:, (i % 2) * 512:(i % 2 + 1) * 512]
            banks.append(t)
        g.tslots = []
        for i in range(2):
            t = Tl(psb)
            t.t = psb[:, i * 1024:(i + 1) * 1024]
            g.tslots.append(t)
        g.ring = banks
        g.pi = 0

        def psum():
            t = g.ring[g.pi % len(g.ring)]
            g.pi += 1
            return t
        g.psum = psum
        kb.dma("sp", cst[:, :], consts[:, :], [], [cst])
        kb.dma("sp", gains[:, 0:32], nmix[:, :], [], [gains])
        kb.dma("sp", gains[:, 32:64], nffn[:, :], [], [gains])
        kb.dma("sp", gains[:, 64:80], fnorm[:, :], [], [gains])
        kb.dma("sp", cw[:, :], convw[:, :], [], [cw])
        kb.dma("sp", sm8[:, 0:1], alog[:, :], [], [sm8])
        kb.dma("sp", sm8[:, 1:2], dtb[:, :], [], [sm8])
        kb.dma("sp", gn[:, :], gnorm[:, :], [], [gn])
        onesb = sb(nc, es, "onesb", [128, 128], BF16)
        g.onesb = onesb.t
        rqb = sb(nc, es, "rqb", [128, 128], BF16)
        rib = sb(nc, es, "rib", [128, 128], BF16)
        g.rqb, g.rib = rqb.t, rib.t
        kb.cp("dve", onesb[:, :], cst[:, C_ONES:C_ONES + 128], [cst], [onesb])
        kb.cp("dve", rqb[:, :], cst[:, C_RQ:C_RQ + 128], [cst], [rqb])
        kb.cp("dve", rib[:, :], cst[:, C_RI:C_RI + 128], [cst], [rib])
        kb.op("dve", lambda eng: eng.memset(small[:, 0:1], EPS), [], [small])
        kb.op("dve", lambda eng: eng.memset(small[:, 1:2], 1.0), [], [small])
        for q4 in range(4):
            kb.dma("sp", g.xres[q4 * 512:(q4 + 1) * 512, :], xT[q4 * 512:(q4 + 1) * 512, :], [], [])
        kb.barrier()

        def run_l0():
            with ExitStack() as es1:
                hT = sb(nc, es1, "hT", [128, 16, S], BF16)
                phase_norm(g, g.xres, gains.t[:, 0:16], hT)
                phase_inproj0(g, hT)
            if stop_after == "inproj0":
                return
            with ExitStack() as es1:
                gtok = sb(nc, es1, "gtok", [64, 32, 8], F32)
                btok = sb(nc, es1, "btok", [64, 32, 8], F32)
                phase_gdn_pre(g, gtok, btok)
                if stop_after == "gdn_pre":
                    return
                phase_gdn_main(g, gtok, btok)
            if stop_after == "gdn":
                return
            phase_sb(g)
            if stop_after == "sb":
                return
            with ExitStack() as es1:
                oTs = sb(nc, es1, "oTs", [128, 16, S], BF16)
                ov = g.oT.rearrange("(c p) s -> p c s", p=128)
                for q4 in range(4):
                    kb.dma("sp", oTs[:, q4 * 4:(q4 + 1) * 4, :], ov[:, q4 * 4:(q4 + 1) * 4, :], [], [oTs])
                phase_residual_dense(g, oTs, 16, 0, S, g.w_out_ab, g.xres, 512)
            if stop_after == "x1":
                return
            with ExitStack() as es1:
                hT = sb(nc, es1, "hT", [128, 16, S], BF16)
                phase_norm(g, g.xres, gains.t[:, 32:48], hT)
                phase_ffn(g, 0, hT)
            phase_ffn2(g, 0)
            if stop_after == "x2":
                return

        def run():
            if not skip_l0:
                run_l0()
                if stop_after in ("inproj0", "gdn_pre", "gdn", "sb", "x1", "x2"):
                    return
            with ExitStack() as es1:
                hT = sb(nc, es1, "hT", [128, 16, S], BF16)
                if not skip_l0:
                    phase_norm(g, g.xres, gains.t[:, 16:32], hT)
                else:
                    phase_norm(g, xT, gains.t[:, 16:32], hT)
                tabs = build_rope_tables(g, es1, pos)
                phase_inproj1(g, hT, tabs)
            if stop_after == "inproj1":
                return
            phase_dsa(g)
            if stop_after == "dsa":
                return
            with ExitStack() as es1:
                oTs = sb(nc, es1, "oTs", [128, 16, S], BF16)
                ov = g.oT.rearrange("(c p) s -> p c s", p=128)
                for q4 in range(4):
                    kb.dma("sp", oTs[:, q4 * 4:(q4 + 1) * 4, :], ov[:, q4 * 4:(q4 + 1) * 4, :], [], [oTs])
                phase_residual_dense(g, oTs, 16, 0, S, g.w_out_c, g.xres, 512)
            if stop_after == "x3":
                return
            with ExitStack() as es1:
                hT = sb(nc, es1, "hT", [128, 16, S], BF16)
                phase_norm(g, g.xres, gains.t[:, 48:64], hT)
                phase_ffn(g, 1, hT)
            phase_ffn2(g, 1)
        run()
        kb.barrier()
        if stop_after is None:
            phase_final_norm(g, g.xres, gains.t[:, 64:80], outT)
        else:
            for q4 in range(4):
                kb.dma("sp", outT[q4 * 512:(q4 + 1) * 512, :], g.xres[q4 * 512:(q4 + 1) * 512, :], [], [])
        kb.barrier()
        block = es.enter_context(nc.Block())

        @block.tensor
        def _(e):
            for th in kb.thunks["pe"]:
                th(e)

        @block.vector
        def _(e):
            for th in kb.thunks["dve"]:
                th(e)

        @block.scalar
        def _(e):
            for th in kb.thunks["act"]:
                th(e)

        @block.gpsimd
        def _(e):
            for th in kb.thunks["pool"]:
                th(e)

        @block.sync
        def _(e):
            for th in kb.thunks["sp"]:
                th(e)
        print("[kernel] recorded ops:", kb.ninst, {e: len(kb.thunks[e]) for e in ENGS})
    return nc


def make_in_maps(inputs):
    consts = build_consts()
    shared = {
        "nmix": np.ascontiguousarray(inputs["norm_mix"].reshape(2, 16, 128).transpose(2, 0, 1).reshape(128, 32)),
        "nffn": np.ascontiguousarray(inputs["norm_ffn"].reshape(2, 16, 128).transpose(2, 0, 1).reshape(128, 32)),
        "fnorm": np.ascontiguousarray(inputs["final_norm"].reshape(16, 128).T),
        "w_in_ab": np.ascontiguousarray(inputs["w_in_ab"][0]),
        "convw": np.ascontiguousarray(inputs["conv_w_a"][0].reshape(4, 24, 128).transpose(2, 1, 0).reshape(128, 96)),
        "alog": np.ascontiguousarray(inputs["a_log"][0].reshape(8, 1)),
        "dtb": np.ascontiguousarray(inputs["dt_bias"][0].reshape(8, 1)),
        "gnorm": np.ascontiguousarray(inputs["gdn_norm"][0].reshape(128, 1)),
        "w_out_ab": np.ascontiguousarray(inputs["w_out_ab"][0]),
        "w_in_c": np.ascontiguousarray(inputs["w_in_c"][0]),
        "w_out_c": np.ascontiguousarray(inputs["w_out_c"][0]),
        "ffn_gate": np.ascontiguousarray(inputs["ffn_gate"].reshape(2 * D, DFF)),
        "ffn_up": np.ascontiguousarray(inputs["ffn_up"].reshape(2 * D, DFF)),
        "ffn_down": np.ascontiguousarray(inputs["ffn_down"].reshape(2 * DFF, D)),
        "consts": consts,
    }
    shared = {k: np.asarray(v, np.float32) for k, v in shared.items()}
    maps = []
    for b in range(8):
        m = dict(shared)
        m["xT"] = np.ascontiguousarray(np.asarray(inputs["x"][b], np.float32).T)
        m["pos"] = np.ascontiguousarray(np.asarray(inputs["positions"][b], np.int32).reshape(1, S))
        maps.append(m)
    return maps


def kernel(**inputs):
    nc = build_program()
    maps = make_in_maps(inputs)
    res = run_bass_kernel_spmd(nc, maps, core_ids=list(range(8)))
    out = np.stack([np.ascontiguousarray(r["outT"].T) for r in res.results], axis=0)
    return out.astype(np.float32)
```

```python
import numpy as np
from contextlib import ExitStack
import concourse.bass as bass
import concourse.mybir as mybir
from concourse.bass_utils import run_bass_kernel_spmd

F32 = mybir.dt.float32
BF16 = mybir.dt.bfloat16
I32 = mybir.dt.int32
AF = mybir.ActivationFunctionType
ALU = mybir.AluOpType

S = 2048
D = 2048
DFF = 5632
EPS = 1e-6
NEG = -1.0e30
NEG16 = -60000.0
F16 = mybir.dt.float16
SAME_ENGINE_SYNC = True
N_DMA_SEMS = 24

AB_IN = 7184
C_IN = 4176

C_IDENT = 0
C_ONES = 128
C_SBTRI = 256
C_SBMSK = 384
C_TRI8 = 512
C_ID8 = 1024
C_MS8 = 1536
C_U = 2048
C_CB = 2112
C_RQ = 2240
C_RI = 2368
C_FQ = 2496
C_FI = 2497
NCONST = 2498


def build_consts():
    c = np.zeros((128, NCONST), np.float32)
    p = np.arange(128)[:, None]
    f = np.arange(128)[None, :]
    c[:, C_IDENT:C_IDENT + 128] = (p == f)
    c[:, C_ONES:C_ONES + 128] = 1.0
    c[:, C_SBTRI:C_SBTRI + 128] = (p > f)
    c[:, C_SBMSK:C_SBMSK + 128] = (f > p)
    p64 = np.arange(64)[:, None]
    i64 = np.arange(64)[None, :]
    for h in range(8):
        c[:64, C_TRI8 + h * 64:C_TRI8 + (h + 1) * 64] = (p64 <= i64)
        c[:64, C_ID8 + h * 64:C_ID8 + (h + 1) * 64] = (p64 == i64)
        c[:64, C_MS8 + h * 64:C_MS8 + (h + 1) * 64] = (p64 < i64)
    c[:64, C_U:C_U + 64] = (p64 > i64)
    c[:, C_CB:C_CB + 128] = np.where(f <= p, 0.0, NEG)
    rq = np.zeros((128, 128), np.float32)
    for m in range(16):
        rq[m + 16, m] = -1.0
        rq[m, m + 16] = 1.0
    c[:, C_RQ:C_RQ + 128] = rq
    ri = np.zeros((128, 128), np.float32)
    for base in (0, 64):
        for m in range(8):
            ri[base + m + 8, base + m] = -1.0
            ri[base + m, base + m + 8] = 1.0
    c[:, C_RI:C_RI + 128] = ri
    fq = np.zeros(128, np.float64)
    for m in range(32):
        fq[m] = 500000.0 ** (-(m % 16) / 16.0)
    fi = np.zeros(128, np.float64)
    for base in (0, 64):
        for m in range(16):
            fi[base + m] = 500000.0 ** (-(m % 8) / 8.0)
    c[:, C_FQ] = fq.astype(np.float32)
    c[:, C_FI] = fi.astype(np.float32)
    return c


class Buf:
    __slots__ = ("w", "r")

    def __init__(self):
        self.w = None
        self.r = []


class Tl:
    __slots__ = ("t", "b")

    def __init__(self, t):
        self.t = t
        self.b = Buf()

    def __getitem__(self, k):
        return self.t[k]


ENGS = ("pe", "dve", "act", "pool", "sp")


class KB:
    def __init__(self, nc, es):
        self.nc = nc
        self.sems = []
        self.thunks = {e: [] for e in ENGS}
        self.cnt = {e: 0 for e in ENGS}
        self.waited = {e: {} for e in ENGS}
        self.esid = {}
        for e in ENGS:
            self.esid[e] = len(self.sems)
            self.sems.append(es.enter_context(nc.semaphore("s_" + e)))
        self.dsid = []
        self.dtot = []
        for i in range(N_DMA_SEMS):
            self.dsid.append(len(self.sems))
            self.sems.append(es.enter_context(nc.semaphore("d%d" % i)))
            self.dtot.append(0)
        self.di = 0
        self.ninst = 0

    def _wait(self, e, tok, raw=True):
        if tok is None:
            return
        sid, val = tok
        if sid == self.esid[e] and (e == "pe" or not SAME_ENGINE_SYNC or not raw):
            return
        if self.waited[e].get(sid, 0) >= val:
            return
        self.waited[e][sid] = val
        sem = self.sems[sid]
        self.thunks[e].append(lambda eng: eng.wait_ge(sem, val))

    def _deps(self, e, reads, writes):
        for b in reads:
            self._wait(e, b.w)
        for b in writes:
            self._wait(e, b.w, raw=False)
            for t in b.r:
                self._wait(e, t, raw=False)

    def _commit(self, tok, reads, writes):
        for b in reads:
            b.r.append(tok)
            if len(b.r) > 64:
                b.r = b.r[-48:]
        for b in writes:
            b.w = tok
            b.r = []

    def op(self, e, fn, reads=(), writes=()):
        reads = [x.b if isinstance(x, Tl) else x for x in reads]
        writes = [x.b if isinstance(x, Tl) else x for x in writes]
        self._deps(e, reads, writes)
        self.cnt[e] += 1
        sid = self.esid[e]
        sem = self.sems[sid]
        self.thunks[e].append(lambda eng: fn(eng).then_inc(sem, 1))
        self._commit((sid, self.cnt[e]), reads, writes)
        self.ninst += 1

    def dma(self, q, out, in_, reads=(), writes=()):
        reads = [x.b if isinstance(x, Tl) else x for x in reads]
        writes = [x.b if isinstance(x, Tl) else x for x in writes]
        i = self.di
        self.di = (i + 1) % N_DMA_SEMS
        sid = self.dsid[i]
        if self.dtot[i] > 0:
            self._wait(q, (sid, self.dtot[i]))
        self._deps(q, reads, writes)
        self.dtot[i] += 16
        sem = self.sems[sid]
        self.thunks[q].append(lambda eng: eng.dma_start(out=out, in_=in_).then_inc(sem, 16))
        self._commit((sid, self.dtot[i]), reads, writes)
        self.ninst += 1

    def barrier(self):
        toks = [(self.esid[e], self.cnt[e]) for e in ENGS if self.cnt[e] > 0]
        toks += [(self.dsid[i], self.dtot[i]) for i in range(N_DMA_SEMS) if self.dtot[i] > 0]
        for e in ENGS:
            for t in toks:
                if t[0] != self.esid[e]:
                    self._wait(e, t)

    def mm(self, out, pairs, reads, writes):
        n = len(pairs)

        def fn(eng):
            ins = None
            for i, (l, r) in enumerate(pairs):
                ins = eng.matmul(out, l, r, start=(i == 0), stop=(i == n - 1))
            return ins
        self.op("pe", fn, reads, writes)

    def mm_multi(self, groups, reads, writes):
        def fn(eng):
            ins = None
            for out, pairs in groups:
                n = len(pairs)
                for i, (l, r) in enumerate(pairs):
                    ins = eng.matmul(out, l, r, start=(i == 0), stop=(i == n - 1))
            return ins
        self.op("pe", fn, reads, writes)

    def tr_multi(self, items, ident, reads, writes):
        def fn(eng):
            ins = None
            for out, in_ in items:
                ins = eng.transpose(out, in_, ident)
            return ins
        self.op("pe", fn, reads, writes)

    def act(self, out, in_, func, reads, writes, bias=None, scale=None, accum=None):
        kw = {}
        if bias is not None:
            kw["bias"] = bias
        if scale is not None:
            kw["scale"] = scale
        if accum is not None:
            kw["accum_out"] = accum
        self.op("act", lambda eng: eng.activation(out=out, in_=in_, func=func, **kw), reads, writes)

    def tt(self, out, in0, in1, op, reads, writes, e="dve"):
        self.op(e, lambda eng: eng.tensor_tensor(out=out, in0=in0, in1=in1, op=op), reads, writes)

    def ts(self, out, in0, s1, s2, op0, op1, reads, writes, e="dve"):
        if op1 is None:
            self.op(e, lambda eng: eng.tensor_scalar(out=out, in0=in0, scalar1=s1, scalar2=None, op0=op0), reads, writes)
        else:
            self.op(e, lambda eng: eng.tensor_scalar(out=out, in0=in0, scalar1=s1, scalar2=s2, op0=op0, op1=op1), reads, writes)

    def stt(self, out, in0, scalar, in1, op0, op1, reads, writes):
        self.op("dve", lambda eng: eng.scalar_tensor_tensor(out=out, in0=in0, scalar=scalar, in1=in1, op0=op0, op1=op1), reads, writes)

    def cp(self, e, out, in_, reads, writes):
        if e == "act":
            self.op("act", lambda eng: eng.copy(out=out, in_=in_), reads, writes)
        else:
            self.op(e, lambda eng: eng.tensor_copy(out=out, in_=in_), reads, writes)


class Ctx:
    pass


_UID = [0]


def sb(nc, es, name, shape, dt):
    _UID[0] += 1
    return Tl(es.enter_context(nc.sbuf_tensor("%s_u%d" % (name, _UID[0]), list(shape), dt)))


def phase_norm(g, xsrc, gain_ap, hT):
    kb, nc = g.kb, g.nc
    with ExitStack() as es:
        xs = [sb(nc, es, "nx%d" % i, [128, 16, 512], F32) for i in range(2)]
        sq = sb(nc, es, "nsq", [128, 16, 512], BF16)
        rs = sb(nc, es, "nrs", [128, 512], F32)
        xv = xsrc.rearrange("(c p) s -> p c s", p=128)
        for t in range(4):
            x = xs[t % 2]
            for q4 in range(4):
                kb.dma("sp", x[:, q4 * 4:(q4 + 1) * 4, :], xv[:, q4 * 4:(q4 + 1) * 4, t * 512:(t + 1) * 512], [], [x])
            kb.act(sq[:, :, :], x[:, :, :], AF.Square, [x], [sq])
            ps = g.psum()
            kb.mm(ps[:, :], [(g.onesb[:, :], sq[:, c, :]) for c in range(16)], [sq], [ps])
            kb.act(rs[:, :], ps[:, :], AF.Ln, [ps], [rs], bias=g.eps[:, 0:1], scale=1.0 / D)
            kb.act(rs[:, :], rs[:, :], AF.Exp, [rs], [rs], scale=-0.5)
            for c in range(16):
                kb.stt(hT[:, c, t * 512:(t + 1) * 512], x[:, c, :], gain_ap[:, c:c + 1], rs[:, :],
                       ALU.mult, ALU.mult, [x, rs], [hT])
        kb.barrier()


def dense(g, XT, KC, toks0, ntok, W, blocks, epi, W2=None, NB=512, tm_epi=None, XTs=None):
    kb, nc = g.kb, g.nc
    with ExitStack() as es:
        nw = 2 if W2 is None else 4
        wbs = [sb(nc, es, "wb%d" % i, [128, KC, NB], BF16) for i in range(nw)]
        Wv = W.rearrange("(c p) n -> p c n", p=128)
        W2v = W2.rearrange("(c p) n -> p c n", p=128) if W2 is not None else None
        kstep = 8 if KC <= 16 else 11
        pending = [None]
        for bi, blk in enumerate(blocks):
            c0, w = blk["c0"], blk["w"]
            if W2 is None:
                wb = wbs[bi % 2]
                wb2 = None
            else:
                wb = wbs[(bi % 2) * 2]
                wb2 = wbs[(bi % 2) * 2 + 1]
            for k0 in range(0, KC, kstep):
                k1 = min(KC, k0 + kstep)
                kb.dma("pool", wb[:, k0:k1, 0:w], Wv[:, k0:k1, c0:c0 + w], [], [wb])
                if wb2 is not None:
                    kb.dma("pool", wb2[:, k0:k1, 0:w], W2v[:, k0:k1, c0:c0 + w], [], [wb2])
            if blk.get("tm"):
                if pending[0] is not None:
                    pending[0]()
                    pending[0] = None
                for tb in range(ntok // 128):
                    ps = g.psum()
                    kb.mm(ps[:, 0:w], [(XT[:, c, tb * 128:(tb + 1) * 128], wb[:, c, 0:w]) for c in range(KC)],
                          [XT, wb], [ps])
                    tm_epi(c0, w, toks0 + tb * 128, ps)
            else:
                for tt in range(ntok // 512):
                    for (off, width, tag) in blk["segs"]:
                        ps = g.psum()
                        if XTs is not None:
                            kb.mm(ps[0:width, :], [(wb[:, c, off:off + width], XTs[tt][:, c, :]) for c in range(KC)],
                                  [XTs[tt], wb], [ps])
                        else:
                            kb.mm(ps[0:width, :], [(wb[:, c, off:off + width], XT[:, c, tt * 512:(tt + 1) * 512]) for c in range(KC)],
                                  [XT, wb], [ps])
                        ps2 = None
                        if wb2 is not None:
                            ps2 = g.psum()
                            kb.mm(ps2[0:width, :], [(wb2[:, c, off:off + width], XT[:, c, tt * 512:(tt + 1) * 512]) for c in range(KC)],
                                  [XT, wb2], [ps2])
                        if pending[0] is not None:
                            pending[0]()
                        pending[0] = epi(tag, c0 + off, width, toks0 + tt * 512, ps, ps2)
        if pending[0] is not None:
            pending[0]()
        kb.barrier()


def make_blocks(segments, NB=512):
    blocks = []
    cur = None
    for (c0, w, tag, tm) in segments:
        if tm:
            blocks.append({"c0": c0, "w": w, "tm": True})
            cur = None
            continue
        if cur is not None and cur["c0"] + cur["w"] == c0 and cur["w"] + w <= NB:
            cur["segs"].append((cur["w"], w, tag))
            cur["w"] += w
        else:
            cur = {"c0": c0, "w": w, "segs": [(0, w, tag)]}
            blocks.append(cur)
    return blocks


class Stager:
    def __init__(self, g, es, name, shape, dt, n=3):
        self.tiles = [sb(g.nc, es, "%s%d" % (name, i), shape, dt) for i in range(n)]
        self.i = 0

    def next(self):
        t = self.tiles[self.i % len(self.tiles)]
        self.i += 1
        return t


def phase_residual_dense(g, XT, KC, toks0, ntok, W, xres, NB, XTs=None):
    kb, nc = g.kb, g.nc
    with ExitStack() as es:
        xin = Stager(g, es, "rxin", [128, 512], F32, 3)
        xout = Stager(g, es, "rxo", [128, 512], F32, 3)

        def epi(tag, c, width, t0, ps, ps2):
            a = xin.next()
            kb.dma("act", a[:, :], xres[c:c + 128, t0:t0 + 512], [], [a])
            o = xout.next()
            kb.tt(o[:, :], ps[:, :], a[:, :], ALU.add, [ps, a], [o])
            kb.dma("sp", xres[c:c + 128, t0:t0 + 512], o[:, :], [o], [])
        segs = [(c * 128, 128, None, False) for c in range(D // 128)]
        dense(g, XT, KC, toks0, ntok, W, make_blocks(segs, NB), epi, NB=NB, XTs=XTs)


def phase_ffn(g, layer, hT):
    kb, nc = g.kb, g.nc
    Wg = g.ffn_gate[layer * D:(layer + 1) * D, :]
    Wu = g.ffn_up[layer * D:(layer + 1) * D, :]
    Wd = g.ffn_down[layer * DFF:(layer + 1) * DFF, :]
    with ExitStack() as es:
        sg = Stager(g, es, "fsg", [128, 512], F32, 2)
        st = Stager(g, es, "fst", [128, 512], BF16, 3)

        def epi(tag, c, width, t0, ps, ps2):
            a = sg.next()
            kb.act(a[:, :], ps[:, :], AF.Silu, [ps], [a])
            o = st.next()
            kb.tt(o[:, :], ps2[:, :], a[:, :], ALU.mult, [ps2, a], [o])
            kb.dma("sp", g.actT[c:c + 128, t0:t0 + 512], o[:, :], [o], [])
        segs = [(c * 128, 128, None, False) for c in range(DFF // 128)]
        dense(g, hT, 16, 0, S, Wg, make_blocks(segs, 512), epi, W2=Wu, NB=512)


def phase_ffn2(g, layer):
    kb, nc = g.kb, g.nc
    Wd = g.ffn_down[layer * DFF:(layer + 1) * DFF, :]
    with ExitStack() as es:
        aTs = [sb(nc, es, "faT%d" % i, [128, 44, 512], BF16) for i in range(2)]
        av = g.actT.rearrange("(c p) s -> p c s", p=128)
        for half in range(2):
            for tt in range(2):
                t0 = half * 1024 + tt * 512
                for k0 in range(0, 44, 4):
                    kb.dma("sp", aTs[tt][:, k0:k0 + 4, :], av[:, k0:k0 + 4, t0:t0 + 512], [], [aTs[tt]])
            phase_residual_dense(g, None, 44, half * 1024, 1024, Wd, g.xres, 256, XTs=aTs)


def phase_inproj0(g, hT):
    kb, nc = g.kb, g.nc
    with ExitStack() as es:
        s32 = Stager(g, es, "i0s", [128, 512], F32, 3)
        s16 = Stager(g, es, "i0h", [128, 512], BF16, 3)

        def epi(tag, c, width, t0, ps, ps2):
            kind, row = tag
            if kind == "p0":
                o = s32.next()
                kb.cp("act", o[0:width, :], ps[0:width, :], [ps], [o])
                kb.dma("sp", g.P0T[row:row + width, t0:t0 + 512], o[0:width, :], [o], [])
            elif kind == "bq":
                o = s16.next()
                kb.act(o[:, :], ps[:, :], AF.Copy, [ps], [o], scale=128.0 ** -0.5)
                kb.dma("sp", g.bqT[row:row + 128, t0:t0 + 512], o[:, :], [o], [])
            else:
                o = s16.next()
                kb.cp("act", o[:, :], ps[:, :], [ps], [o])
                kb.dma("sp", g.bkT[row:row + 128, t0:t0 + 512], o[:, :], [o], [])

        def tm_epi(c0, w, t0, ps):
            o = s16.next()
            kb.cp("act", o[:, 0:w], ps[:, 0:w], [ps], [o])
            kb.dma("sp", g.bv_tok[t0:t0 + 128, c0 - 6160:c0 - 6160 + w], o[:, 0:w], [o], [])
        segs = []
        for c in range(32):
            segs.append((c * 128, 128, ("p0", c * 128), False))
        segs.append((4096, 16, ("p0", 4096), False))
        for c in range(8):
            segs.append((4112 + c * 128, 128, ("bq", c * 128), False))
        for c in range(8):
            segs.append((5136 + c * 128, 128, ("bk", c * 128), False))
        segs.append((6160, 512, None, True))
        segs.append((6672, 512, None, True))
        dense(g, hT, 16, 0, S, g.w_in_ab, make_blocks(segs, 512), epi, NB=512, tm_epi=tm_epi)


def phase_sb(g):
    kb, nc = g.kb, g.nc
    with ExitStack() as es:
        qTs_ = [sb(nc, es, "sbq%d" % i, [128, S], BF16) for i in range(2)]
        kTs_ = [sb(nc, es, "sbk%d" % i, [128, S], BF16) for i in range(2)]
        vts_ = [sb(nc, es, "sbv%d" % i, [128, 16, 128], BF16) for i in range(2)]
        zl = sb(nc, es, "sbzl", [128, 16, 512], F32)
        spH = sb(nc, es, "sbsh", [128, 16, 512], BF16)
        spL = sb(nc, es, "sbsl", [128, 16, 512], BF16)
        AT = sb(nc, es, "sbat", [128, 16, 512], BF16)
        e1 = Stager(g, es, "sbe1", [128, 512], F32, 2)
        spf = Stager(g, es, "sbsp", [128, 512], F32, 2)
        ee = Stager(g, es, "sbee", [128, 512], F32, 2)
        ost = Stager(g, es, "sbos", [128, 512], BF16, 2)
        tri = sb(nc, es, "sbtri", [128, 128], BF16)
        onb = sb(nc, es, "sbone", [128, 128], BF16)
        mkb = sb(nc, es, "sbmkb", [128, 128], BF16)
        kb.cp("dve", tri[:, :], g.cst[:, C_SBTRI:C_SBTRI + 128], [], [tri])
        kb.cp("dve", onb[:, :], g.cst[:, C_ONES:C_ONES + 128], [], [onb])
        kb.cp("dve", mkb[:, :], g.cst[:, C_SBMSK:C_SBMSK + 128], [], [mkb])
        mkf = g.cst[:, C_SBMSK:C_SBMSK + 128]
        bankR = g.ring.pop()
        bvv = g.bv_tok.rearrange("(n p) c -> p n c", p=128)
        def sb_load(h):
            kb.dma("sp", qTs_[h % 2][:, :], g.bqT[h * 128:(h + 1) * 128, :], [], [qTs_[h % 2]])
            kb.dma("sp", kTs_[h % 2][:, :], g.bkT[h * 128:(h + 1) * 128, :], [], [kTs_[h % 2]])
            kb.dma("sp", vts_[h % 2][:, :, :], bvv[:, :, h * 128:(h + 1) * 128], [], [vts_[h % 2]])
        sb_load(0)
        for h in range(8):
            qT, kT, vt = qTs_[h % 2], kTs_[h % 2], vts_[h % 2]
            if h + 1 < 8:
                sb_load(h + 1)
            for T in range(4):
                nkb = 4 * (T + 1)

                def rng(kbi):
                    off = 0 if kbi < 4 * T else (kbi - 4 * T) * 128
                    return off, 512 - off
                for kbi in range(nkb):
                    off, w = rng(kbi)
                    ps = g.psum()
                    kb.mm(ps[:, off:512], [(kT[:, kbi * 128:(kbi + 1) * 128], qT[:, T * 512 + off:(T + 1) * 512])], [qT, kT], [ps])
                    a = e1.next()
                    kb.act(a[:, off:512], ps[:, off:512], AF.Exp, [ps], [a])
                    s_ = spf.next()
                    kb.act(s_[:, off:512], a[:, off:512], AF.Ln, [a], [s_], bias=g.one[:, 0:1])
                    kb.tt(zl[:, kbi, off:512], ps[:, off:512], s_[:, off:512], ALU.subtract, [ps, s_], [zl])
                    if kbi >= 4 * T:
                        kb.tt(s_[:, off:off + 128], s_[:, off:off + 128], mkf, ALU.mult, [s_], [s_])
                    kb.cp("act", spH[:, kbi, off:512], s_[:, off:512], [s_], [spH])
                    kb.tt(spL[:, kbi, off:512], s_[:, off:512], spH[:, kbi, off:512], ALU.subtract, [s_, spH], [spL])
                for kbi in range(4 * T + 1, nkb):
                    off, w = rng(kbi)
                    kb.op("pool", lambda eng, o=spH[:, kbi, 0:off]: eng.memset(o, 0.0), [], [spH])
                    kb.op("pool", lambda eng, o=spL[:, kbi, 0:off]: eng.memset(o, 0.0), [], [spL])

                def emitB(kbi):
                    off, w = rng(kbi)
                    psB = g.psum()
                    kb.mm(psB[:, off:512], [(tri[:, :], spH[:, kbi, off:512]), (tri[:, :], spL[:, kbi, off:512])], [spH, spL, tri], [psB])
                    return psB
                psB = emitB(nkb - 1)
                for kbi in range(nkb - 1, -1, -1):
                    off, w = rng(kbi)
                    psBn = emitB(kbi - 1) if kbi > 0 else None
                    e_ = ee.next()
                    kb.tt(e_[:, off:512], zl[:, kbi, off:512], psB[:, off:512], ALU.subtract, [zl, psB], [e_])
                    if kbi < nkb - 1:
                        kb.tt(e_[:, off:512], e_[:, off:512], bankR[:, off:512], ALU.subtract, [e_, bankR], [e_])
                    kb.act(AT[:, kbi, off:512], e_[:, off:512], AF.Exp, [e_], [AT])
                    if kbi >= 4 * T:
                        kb.tt(AT[:, kbi, off:off + 128], AT[:, kbi, off:off + 128], mkb[:, :], ALU.mult, [AT, mkb], [AT])
                    if kbi > 0:
                        first = (kbi == nkb - 1)

                        def fnR(eng, kbi=kbi, first=first):
                            eng.matmul(bankR[:, :], onb[:, :], spH[:, kbi, :], start=first, stop=False)
                            return eng.matmul(bankR[:, :], onb[:, :], spL[:, kbi, :], start=False, stop=True)
                        kb.op("pe", fnR, [spH, spL, onb], [bankR])
                    psB = psBn
                ps = g.psum()
                pv = []
                for kbi in range(nkb):
                    off, w = rng(kbi)
                    pv.append((ps[:, off:512], vt[:, kbi, :], AT[:, kbi, off:512]))

                def fn2(eng, pv=pv):
                    ins = None
                    n = len(pv)
                    for i, (o, l, r) in enumerate(pv):
                        ins = eng.matmul(o, l, r, start=(i == 0), stop=(i == n - 1))
                    return ins
                kb.op("pe", fn2, [vt, AT], [ps])
                o = ost.next()
                kb.cp("act", o[:, :], ps[:, :], [ps], [o])
                kb.dma("pool", g.oT[1024 + h * 128:1024 + (h + 1) * 128, T * 512:(T + 1) * 512], o[:, :], [o], [])
        kb.barrier()
        g.ring.append(bankR)


def phase_gdn_pre(g, gtok, btok):
    kb, nc = g.kb, g.nc
    cst = g.cst
    ident = cst[:, C_IDENT:C_IDENT + 128]
    with ExitStack() as es:
        xps = [sb(nc, es, "gxp%d" % i, [128, S + 3], F32) for i in range(2)]
        accs = [sb(nc, es, "gacc%d" % i, [128, S], F32) for i in range(2)]
        ys = [sb(nc, es, "gy%d" % i, [128, S], F32) for i in range(2)]
        ybs = [sb(nc, es, "gyb%d" % i, [128, S], BF16) for i in range(2)]
        sqs = [sb(nc, es, "gsq%d" % i, [128, S], BF16) for i in range(2)]
        rns = [sb(nc, es, "grn%d" % i, [128, S], F32) for i in range(2)]
        stgs = [sb(nc, es, "gstg%d" % i, [64, 32, 128], F32) for i in range(2)]
        for xp in xps:
            kb.op("dve", lambda eng, xp=xp: eng.memset(xp[:, 0:3], 0.0), [], [xp])
        for ci in range(24):
            kind, h = ci // 8, ci % 8
            xp, acc, y, yb, sq, rn, stg = xps[ci % 2], accs[ci % 2], ys[ci % 2], ybs[ci % 2], sqs[ci % 2], rns[ci % 2], stgs[ci % 2]
            kb.dma("sp", xp[:, 3:3 + S], g.P0T[ci * 128:(ci + 1) * 128, :], [], [xp])
            kb.ts(acc[:, :], xp[:, 0:S], g.convw[:, ci * 4:ci * 4 + 1], None, ALU.mult, None, [xp], [acc])
            for i in range(1, 4):
                kb.stt(acc[:, :], xp[:, i:i + S], g.convw[:, ci * 4 + i:ci * 4 + i + 1], acc[:, :], ALU.mult, ALU.add, [xp, acc], [acc])
            kb.act(y[:, :], acc[:, :], AF.Silu, [acc], [y])
            if kind < 2:
                kb.act(sq[:, :], y[:, :], AF.Square, [y], [sq])
                for tt in range(4):
                    ps = g.psum()
                    kb.mm(ps[:, :], [(g.onesb[:, :], sq[:, tt * 512:(tt + 1) * 512])], [sq], [ps])
                    kb.act(rn[:, tt * 512:(tt + 1) * 512], ps[:, :], AF.Ln, [ps], [rn], bias=g.eps[:, 0:1])
                kb.act(rn[:, :], rn[:, :], AF.Exp, [rn], [rn], scale=-0.5)
                sc = (128.0 ** -0.5) if kind == 0 else 1.0
                if kind == 0:
                    kb.stt(yb[:, :], y[:, :], sc, rn[:, :], ALU.mult, ALU.mult, [y, rn], [yb])
                else:
                    kb.stt(y[:, :], y[:, :], sc, rn[:, :], ALU.mult, ALU.mult, [y, rn], [y])
                    kb.cp("act", yb[:, :], y[:, :], [y], [yb])
                dst = g.gqT if kind == 0 else g.gkT
                kb.dma("pool", dst[h * 128:(h + 1) * 128, :], yb[:, :], [yb], [])
            if kind >= 1:
                for n4 in range(8):
                    ps = g.psum()
                    kb.tr_multi([(ps[0:64, j * 128:(j + 1) * 128], y[:, (n4 * 4 + j) * 64:(n4 * 4 + j + 1) * 64]) for j in range(4)],
                                ident, [y], [ps])
                    kb.cp("act", stg[:, n4 * 4:(n4 + 1) * 4, :], ps[0:64, :].rearrange("p (a b) -> p a b", a=4), [ps], [stg])
                dst = g.gk_tok if kind == 1 else g.gv_tok
                kb.dma("pool", dst.rearrange("(n j) c -> j n c", j=64)[:, :, h * 128:(h + 1) * 128], stg[:, :, :], [stg], [])
        al = sb(nc, es, "gal", [8, S], F32)
        be = sb(nc, es, "gbe", [8, S], F32)
        small = sb(nc, es, "gsm", [8, 4], F32)
        kb.dma("sp", al[:, :], g.P0T[4096:4104, :], [], [al])
        kb.dma("sp", be[:, :], g.P0T[4104:4112, :], [], [be])
        kb.act(small[:, 0:1], g.alog[:, 0:1], AF.Exp, [], [small])
        kb.ts(small[:, 1:2], small[:, 0:1], -1.0, None, ALU.mult, None, [small], [small])
        kb.act(al[:, :], al[:, :], AF.Exp, [al], [al], bias=g.dtb[:, 0:1])
        kb.act(al[:, :], al[:, :], AF.Ln, [al], [al], bias=g.one[0:8, 0:1])
        kb.ts(al[:, :], al[:, :], small[:, 1:2], None, ALU.mult, None, [al, small], [al])
        kb.act(be[:, :], be[:, :], AF.Sigmoid, [be], [be])
        for src, dstt in ((al, gtok), (be, btok)):
            ps = g.psum()
            kb.tr_multi([(ps[0:64, n * 8:(n + 1) * 8], src[0:8, n * 64:(n + 1) * 64]) for n in range(32)],
                        cst[0:8, C_IDENT:C_IDENT + 8], [src], [ps])
            kb.cp("act", dstt[:, :, :], ps[0:64, 0:256].rearrange("p (a b) -> p a b", a=32), [ps], [dstt])
        kb.barrier()


def phase_gdn_main(g, gtok, btok):
    kb, nc = g.kb, g.nc
    cst = g.cst
    H = 8
    tri8 = cst[0:64, C_TRI8:C_TRI8 + 512]
    id8 = cst[0:64, C_ID8:C_ID8 + 512]
    ms8 = cst[0:64, C_MS8:C_MS8 + 512]
    U = cst[0:64, C_U:C_U + 64]
    triI = cst[0:64, C_TRI8:C_TRI8 + 64]
    id64 = cst[0:64, C_IDENT:C_IDENT + 64]
    ones = g.ones
    with ExitStack() as es:
        def T64(name, w=512):
            return sb(nc, es, name, [64, w], F32)
        qTs = [sb(nc, es, "mq%d" % i, [128, 8, 256], BF16) for i in range(2)]
        kTs = [sb(nc, es, "mk%d" % i, [128, 8, 256], BF16) for i in range(2)]
        kts = [sb(nc, es, "mkt%d" % i, [64, 1024], F32) for i in range(2)]
        vts = [sb(nc, es, "mvt%d" % i, [64, 1024], F32) for i in range(2)]
        G2, B2, decT, M1, M2 = T64("mG2"), T64("mB2"), T64("mdec"), T64("mM1"), T64("mM2")
        AT = sb(nc, es, "mAT", [64, 512], BF16)
        Qb = [sb(nc, es, "mQ%d" % i, [64, 512], BF16) for i in range(2)]
        Pb = [sb(nc, es, "mP%d" % i, [64, 512], BF16) for i in range(2)]
        Rb = [T64("mR0"), T64("mR1")]
        Rbb = [sb(nc, es, "mRb%d" % i, [64, 512], BF16) for i in range(2)]
        Q0f = T64("mQ0f")
        egbc = sb(nc, es, "megbc", [128, 512], F32)
        esm = T64("mesm", 16)
        bg = T64("mbg", 8)
        u = T64("mu", 1024)
        vb, kbg, kdec, vnew = [sb(nc, es, nm, [64, 1024], BF16) for nm in ("mvb", "mkbg", "mkdec", "mvn")]
        qg = sb(nc, es, "mqg", [128, 512], BF16)
        wT = sb(nc, es, "mwT", [128, 512], BF16)
        St = sb(nc, es, "mS", [128, 1024], F32)
        Stb = sb(nc, es, "mSb", [128, 1024], BF16)
        oraw = sb(nc, es, "mor", [128, 8, 256], F32)
        osq = sb(nc, es, "mosq", [128, 8, 256], BF16)
        gate = sb(nc, es, "mgate", [128, 8, 256], F32)
        rsd = sb(nc, es, "mrsd", [128, 8, 256], F32)
        oout = sb(nc, es, "moo", [128, 8, 256], BF16)
        kb.op("dve", lambda eng: eng.memset(St[:, :], 0.0), [], [St])
        gqv = g.gqT.rearrange("(h p) s -> p h s", p=128)
        gkv = g.gkT.rearrange("(h p) s -> p h s", p=128)
        ktv = g.gk_tok.rearrange("(n j) c -> j n c", j=64)
        vtv = g.gv_tok.rearrange("(n j) c -> j n c", j=64)
        gatev = g.P0T[3072:4096, :].rearrange("(h p) s -> p h s", p=128)
        oTv = g.oT[0:1024, :].rearrange("(h p) s -> p h s", p=128)

        def v3(t, w=64):
            return t[:, :].rearrange("p (h i) -> p h i", h=H)

        def load_group(gi):
            b = gi % 2
            kb.dma("sp", qTs[b][:, :, :], gqv[:, :, gi * 256:(gi + 1) * 256], [], [qTs[b]])
            kb.dma("sp", kTs[b][:, :, :], gkv[:, :, gi * 256:(gi + 1) * 256], [], [kTs[b]])
        load_group(0)
        for n in range(32):
            gi, nl = n // 4, n % 4
            b = gi % 2
            if nl == 0 and gi + 1 < 8:
                load_group(gi + 1)
            if nl == 0:
                kb.dma("sp", gate[:, :, :], gatev[:, :, gi * 256:(gi + 1) * 256], [], [gate])
            qT, kT, kt, vt = qTs[b], kTs[b], kts[n % 2], vts[n % 2]
            kb.dma("sp", kt[:, :], ktv[:, n, :], [], [kt])
            kb.dma("sp", vt[:, :], vtv[:, n, :], [], [vt])
            cs = slice(nl * 64, (nl + 1) * 64)
            gcol = gtok[:, n, :]
            bcol = btok[:, n, :]
            kb.tt(v3(G2), tri8.rearrange("p (h i) -> p h i", h=H), gcol.unsqueeze(2).to_broadcast([64, 8, 64]), ALU.mult, [gtok], [G2])
            kb.tt(v3(B2), id8.rearrange("p (h i) -> p h i", h=H), bcol.unsqueeze(2).to_broadcast([64, 8, 64]), ALU.mult, [btok], [B2])
            psD, psB, psG, psS = g.psum(), g.psum(), g.psum(), g.psum()
            kb.mm(psD[0:64, :], [(U, G2[:, :])], [G2], [psD])
            kb.mm(psB[0:64, :], [(ones[0:64, 0:64], B2[:, :])], [B2], [psB])
            kb.mm(psG[:, :], [(ones[0:64, :], G2[:, :])], [G2], [psG])
            kb.mm_multi([(psS[0:64, 0:8], [(triI, gcol)]), (psS[0:64, 8:16], [(U, gcol)])], [gtok], [psS])
            kb.act(decT[:, :], psD[0:64, :], AF.Exp, [psD], [decT])
            kb.act(egbc[:, :], psG[:, :], AF.Exp, [psG], [egbc])
            kb.act(esm[:, :], psS[0:64, 0:16], AF.Exp, [psS], [esm])
            kb.tt(M1[:, :], decT[:, :], tri8, ALU.mult, [decT], [M1])
            kb.tt(M2[:, :], decT[:, :], ms8, ALU.mult, [decT], [M2])
            kb.tt(M2[:, :], psB[0:64, :], M2[:, :], ALU.mult, [psB, M2], [M2])
            pskk, psqk = g.psum(), g.psum()
            kb.mm_multi([(pskk[0:64, h * 64:(h + 1) * 64], [(kT[:, h, cs], kT[:, h, cs])]) for h in range(H)], [kT], [pskk])
            kb.mm_multi([(psqk[0:64, h * 64:(h + 1) * 64], [(kT[:, h, cs], qT[:, h, cs])]) for h in range(H)], [kT, qT], [psqk])
            kb.tt(AT[:, :], psqk[0:64, :], M1[:, :], ALU.mult, [psqk, M1], [AT])
            Q, P_, R, Rh = Qb[0], Pb[0], Rb[0], Rbb[0]
            kb.stt(Q0f[:, :], pskk[0:64, :], -1.0, M2[:, :], ALU.mult, ALU.mult, [pskk, M2], [Q0f])
            psT = g.psum()
            kb.tr_multi([(psT[0:64, h * 64:(h + 1) * 64], Q0f[:, h * 64:(h + 1) * 64]) for h in range(H)], id64, [Q0f], [psT])
            kb.cp("act", P_[:, :], psT[0:64, :], [psT], [P_])
            kb.cp("act", Q[:, :], Q0f[:, :], [Q0f], [Q])
            kb.tt(R[:, :], Q0f[:, :], id8, ALU.add, [Q0f], [R])
            kb.cp("act", Rh[:, :], R[:, :], [R], [Rh])
            for lv in range(1, 6):
                Qn, Pn, Rn, Rhn = Qb[lv % 2], Pb[lv % 2], Rb[lv % 2], Rbb[lv % 2]
                psP = g.psum()
                kb.mm_multi([(psP[0:64, h * 64:(h + 1) * 64], [(Q[:, h * 64:(h + 1) * 64], P_[:, h * 64:(h + 1) * 64])]) for h in range(H)], [Q, P_], [psP])
                if lv < 5:
                    psQ = g.psum()
                    kb.mm_multi([(psQ[0:64, h * 64:(h + 1) * 64], [(P_[:, h * 64:(h + 1) * 64], Q[:, h * 64:(h + 1) * 64])]) for h in range(H)], [Q, P_], [psQ])
                kb.cp("act", Pn[:, :], psP[0:64, :], [psP], [Pn])
                if lv < 5:
                    kb.cp("dve", Qn[:, :], psQ[0:64, :], [psQ], [Qn])
                psR = g.psum()
                kb.mm_multi([(psR[0:64, h * 64:(h + 1) * 64], [(Pn[:, h * 64:(h + 1) * 64], Rh[:, h * 64:(h + 1) * 64])]) for h in range(H)], [Pn, Rh], [psR])
                kb.tt(Rn[:, :], psR[0:64, :], R[:, :], ALU.add, [psR, R], [Rn])
                kb.cp("act", Rhn[:, :], Rn[:, :], [Rn], [Rhn])
                Q, P_, R, Rh = Qn, Pn, Rn, Rhn
            ktc = kt[:, :].rearrange("p (h d) -> p h d", h=H)
            vtc = vt[:, :].rearrange("p (h d) -> p h d", h=H)
            kb.tt(vb[:, :].rearrange("p (h d) -> p h d", h=H), vtc, bcol.unsqueeze(2).to_broadcast([64, 8, 128]), ALU.mult, [vt, btok], [vb])
            kb.tt(bg[:, :], bcol, esm[:, 0:8], ALU.mult, [btok, esm], [bg])
            kb.tt(kbg[:, :].rearrange("p (h d) -> p h d", h=H), ktc, bg[:, :].unsqueeze(2).to_broadcast([64, 8, 128]), ALU.mult, [kt, bg], [kbg])
            kb.tt(kdec[:, :].rearrange("p (h d) -> p h d", h=H), ktc, esm[:, 8:16].unsqueeze(2).to_broadcast([64, 8, 128]), ALU.mult, [kt, esm], [kdec])
            kb.tt(qg[:, :].rearrange("p (h i) -> p h i", h=H), qT[:, :, cs], egbc[:, :].rearrange("p (h i) -> p h i", h=H), ALU.mult, [qT, egbc], [qg])
            psu0, psu1, psw = g.psum(), g.psum(), g.psum()
            kb.mm_multi([((psu0 if h < 4 else psu1)[0:64, (h % 4) * 128:(h % 4 + 1) * 128], [(Rh[:, h * 64:(h + 1) * 64], vb[:, h * 128:(h + 1) * 128])]) for h in range(H)],
                        [Rh, vb], [psu0, psu1])
            kb.mm_multi([(psw[:, h * 64:(h + 1) * 64], [(kbg[:, h * 128:(h + 1) * 128], Rh[:, h * 64:(h + 1) * 64])]) for h in range(H)], [kbg, Rh], [psw])
            kb.cp("act", u[:, 0:512], psu0[0:64, :], [psu0], [u])
            kb.cp("act", u[:, 512:1024], psu1[0:64, :], [psu1], [u])
            kb.cp("act", wT[:, :], psw[:, :], [psw], [wT])
            kb.cp("act", Stb[:, :], St[:, :], [St], [Stb])
            pws0, pws1 = g.psum(), g.psum()
            kb.mm_multi([((pws0 if h < 4 else pws1)[0:64, (h % 4) * 128:(h % 4 + 1) * 128], [(wT[:, h * 64:(h + 1) * 64], Stb[:, h * 128:(h + 1) * 128])]) for h in range(H)],
                        [wT, Stb], [pws0, pws1])
            kb.tt(vnew[:, 0:512], u[:, 0:512], pws0[0:64, :], ALU.subtract, [u, pws0], [vnew])
            kb.tt(vnew[:, 512:1024], u[:, 512:1024], pws1[0:64, :], ALU.subtract, [u, pws1], [vnew])
            pso = g.psum()
            kb.mm_multi([(pso[:, h * 64:(h + 1) * 64], [(Stb[:, h * 128:(h + 1) * 128], qg[:, h * 64:(h + 1) * 64]),
                                                         (vnew[:, h * 128:(h + 1) * 128], AT[:, h * 64:(h + 1) * 64])]) for h in range(H)],
                        [Stb, qg, vnew, AT], [pso])
            kb.cp("act", oraw[:, :, cs], pso[:, :].rearrange("p (h i) -> p h i", h=H), [pso], [oraw])
            psu0, psu1 = g.psum(), g.psum()
            kb.mm_multi([((psu0 if h < 4 else psu1)[:, (h % 4) * 128:(h % 4 + 1) * 128], [(kdec[:, h * 128:(h + 1) * 128], vnew[:, h * 128:(h + 1) * 128])]) for h in range(H)],
                        [kdec, vnew], [psu0, psu1])
            kb.tt(St[:, :].rearrange("p (h d) -> p h d", h=H), St[:, :].rearrange("p (h d) -> p h d", h=H),
                  egbc[:, :].rearrange("p (h i) -> p h i", h=H)[:, :, 63:64].to_broadcast([128, 8, 128]), ALU.mult, [St, egbc], [St])
            kb.tt(St[:, 0:512], St[:, 0:512], psu0[:, :], ALU.add, [St, psu0], [St])
            kb.tt(St[:, 512:1024], St[:, 512:1024], psu1[:, :], ALU.add, [St, psu1], [St])
            if nl == 3:
                kb.act(osq[:, :, :], oraw[:, :, :], AF.Square, [oraw], [osq])
                for q4 in range(4):
                    ps = g.psum()
                    kb.mm(ps[:, :], [(g.onesb[:, :], osq[:, 2 * q4:2 * q4 + 2, :])], [osq], [ps])
                    kb.act(rsd[:, 2 * q4:2 * q4 + 2, :], ps[:, :].rearrange("p (a b) -> p a b", a=2), AF.Ln, [ps], [rsd], bias=g.eps[:, 0:1], scale=1.0 / 128)
                kb.act(rsd[:, :, :], rsd[:, :, :], AF.Exp, [rsd], [rsd], scale=-0.5)
                kb.act(gate[:, :, :], gate[:, :, :], AF.Silu, [gate], [gate])
                kb.stt(oraw[:, :, :], oraw[:, :, :], g.gnorm[:, 0:1], rsd[:, :, :], ALU.mult, ALU.mult, [oraw, rsd], [oraw])
                kb.tt(oout[:, :, :], oraw[:, :, :], gate[:, :, :], ALU.mult, [oraw, gate], [oout])
                kb.dma("pool", oTv[:, :, gi * 256:(gi + 1) * 256], oout[:, :, :], [oout], [])
        kb.barrier()


PI = 3.14159265358979
PI_LO = 3.1415925


def build_rope_tables(g, es, pos):
    kb, nc = g.kb, g.nc
    tabs = {}
    for nm in ("q", "i"):
        for which in ("sin", "cos"):
            tabs[nm + which] = sb(nc, es, "rt" + nm + which, [128, S], F32)
    with ExitStack() as tes:
        posi = sb(nc, tes, "rpi", [128, S], I32)
        posf = sb(nc, tes, "rpf", [128, S], F32)
        a = sb(nc, tes, "rpa", [128, S], F32)
        ni = sb(nc, tes, "rpn", [128, S], I32)
        nf = sb(nc, tes, "rpnf", [128, S], F32)
        m = sb(nc, tes, "rpm", [128, S], F32)
        kb.dma("sp", posi[:, :], pos.partition_broadcast(128)[:, 0, :], [], [posi])
        kb.cp("dve", posf[:, :], posi[:, :], [posi], [posf])
        for nm, fcol in (("q", C_FQ), ("i", C_FI)):
            for which, shift in (("sin", 0.0), ("cos", PI / 2)):
                out = tabs[nm + which]
                kb.ts(a[:, :], posf[:, :], g.cst[:, fcol:fcol + 1], shift, ALU.mult, ALU.add, [posf], [a])
                kb.ts(ni[:, :], a[:, :], 1.0 / (2 * PI), None, ALU.mult, None, [a], [ni])
                kb.cp("dve", nf[:, :], ni[:, :], [ni], [nf])
                kb.stt(a[:, :], nf[:, :], -2 * PI, a[:, :], ALU.mult, ALU.add, [nf, a], [a])
                kb.ts(m[:, :], a[:, :], PI, None, ALU.is_gt, None, [a], [m])
                kb.stt(a[:, :], m[:, :], -2 * PI, a[:, :], ALU.mult, ALU.add, [m, a], [a])
                kb.ts(m[:, :], a[:, :], -PI, None, ALU.is_lt, None, [a], [m])
                kb.stt(a[:, :], m[:, :], 2 * PI, a[:, :], ALU.mult, ALU.add, [m, a], [a])
                kb.ts(a[:, :], a[:, :], PI_LO, -PI_LO, ALU.min, ALU.max, [a], [a])
                kb.act(out[:, :], a[:, :], AF.Sin, [a], [out])
        kb.barrier()
    return tabs


def phase_inproj1(g, hT, tabs):
    kb, nc = g.kb, g.nc
    with ExitStack() as es:
        s32 = Stager(g, es, "i1s", [128, 512], F32, 3)
        s16 = Stager(g, es, "i1h", [128, 512], BF16, 3)
        xs_ = Stager(g, es, "i1x", [128, 512], F32, 3)
        xb_ = Stager(g, es, "i1xb", [128, 512], BF16, 3)
        t1_ = Stager(g, es, "i1a", [128, 512], F32, 2)
        t2_ = Stager(g, es, "i1b", [128, 512], F32, 2)

        def epi(tag, c, width, t0, ps, ps2):
            kind, row = tag
            if kind == "wi":
                o = s32.next()
                kb.act(o[0:width, :], ps[0:width, :], AF.Copy, [ps], [o], scale=(16 ** -0.5) * (64 ** -0.5))
                kb.dma("sp", g.cwiT[0:width, t0:t0 + 512], o[0:width, :], [o], [])
                return
            isq = kind in ("q", "k")
            R = (g.rqb if isq else g.rib)[0:width, 0:width]
            cos = tabs["qcos" if isq else "icos"]
            sin = tabs["qsin" if isq else "isin"]
            sc = 128.0 ** -0.5 if kind == "q" else 1.0
            x = xs_.next()
            kb.cp("act", x[0:width, :], ps[0:width, :], [ps], [x])
            xb = xb_.next()
            kb.cp("act", xb[0:width, :], ps[0:width, :], [ps], [xb])

            def late():
                pr = g.psum()
                kb.mm(pr[0:width, :], [(R, xb[0:width, :])], [xb], [pr])
                t1 = t1_.next()
                t2 = t2_.next()
                kb.stt(t1[0:width, :], x[0:width, :], sc, cos[0:width, t0:t0 + 512], ALU.mult, ALU.mult, [x, cos], [t1])
                kb.stt(t2[0:width, :], pr[0:width, :], sc, sin[0:width, t0:t0 + 512], ALU.mult, ALU.mult, [pr, sin], [t2])
                if kind in ("q", "k"):
                    o = s16.next()
                    dst = g.cqT if kind == "q" else g.ckT
                else:
                    o = s32.next()
                    dst = g.cqiT if kind == "qi" else g.ckiT
                kb.tt(o[0:width, :], t1[0:width, :], t2[0:width, :], ALU.add, [t1, t2], [o])
                kb.dma("sp", dst[row:row + width, t0:t0 + 512], o[0:width, :], [o], [])
            return late

        def tm_epi(c0, w, t0, ps):
            o = s16.next()
            kb.cp("act", o[:, 0:w], ps[:, 0:w], [ps], [o])
            kb.dma("sp", g.cv_tok[t0:t0 + 128, 0:w], o[:, 0:w], [o], [])
        segs = []
        for c in range(16):
            segs.append((c * 128, 128, ("q", c * 128), False))
        for c in range(4):
            segs.append((2048 + c * 128, 128, ("k", c * 128), False))
        segs.append((2560, 512, None, True))
        for c in range(8):
            segs.append((3072 + c * 128, 128, ("qi", c * 128), False))
        segs.append((4096, 64, ("ki", 0), False))
        segs.append((4160, 16, ("wi", 0), False))
        dense(g, hT, 16, 0, S, g.w_in_c, make_blocks(segs, 512), epi, NB=512, tm_epi=tm_epi)


def phase_dsa(g):
    kb, nc = g.kb, g.nc
    cst = g.cst
    ident = cst[:, C_IDENT:C_IDENT + 128]
    CB = cst[:, C_CB:C_CB + 128]
    with ExitStack() as es:
        ki2 = sb(nc, es, "dki2", [128, S], F32)
        kiH2 = sb(nc, es, "dkiH", [128, S], BF16)
        kiL2 = sb(nc, es, "dkiL", [128, S], BF16)
        kT = sb(nc, es, "dk", [128, 4, S], BF16)
        vt = sb(nc, es, "dv", [128, 16, 512], BF16)
        wiT = sb(nc, es, "dwiT", [16, S], F32)
        witok = sb(nc, es, "dwit", [128, 16, 16], F32)
        wabs = sb(nc, es, "dwa", [128, 16, 16], F32)
        wsgn = sb(nc, es, "dws", [128, 16, 16], F32)
        qi2 = sb(nc, es, "dqi2", [128, 16, 128], F32)
        qib = [sb(nc, es, "dqi%d" % i, [128, 16, 128], BF16) for i in range(2)]
        qb = [sb(nc, es, "dq%d" % i, [128, 16, 128], BF16) for i in range(2)]
        score = sb(nc, es, "dsc", [128, S], F32)
        score1 = sb(nc, es, "dsc1", [128, S], F32)
        mbb1 = sb(nc, es, "dmbb1", [128, S], BF16)
        work = sb(nc, es, "dwk", [128, S], F32)
        mb = sb(nc, es, "dmb", [128, S], F32)
        mbb = sb(nc, es, "dmbb", [128, S], BF16)
        identb = sb(nc, es, "didb", [128, 128], BF16)
        Pms = [sb(nc, es, "dP%d" % i, [128, S], BF16) for i in range(2)]
        PTs = [sb(nc, es, "dPT%d" % i, [128, 16, 128], BF16) for i in range(2)]
        sts = [sb(nc, es, "dst%d" % i, [128, 16], F32) for i in range(2)]
        oblk = sb(nc, es, "dob", [128, 16, 128], F32)
        oTb = sb(nc, es, "doT", [128, 16, 128], BF16)
        m8 = sb(nc, es, "dm8", [128, 8], F32)
        rl_ = Stager(g, es, "drl", [128, 512], F32, 3)
        g.tsi = 0
        kb.cp("dve", identb[:, :], cst[:, C_IDENT:C_IDENT + 128], [], [identb])
        kb.dma("sp", ki2[0:64, :], g.ckiT[:, :], [], [ki2])
        kb.dma("sp", ki2[64:128, :], g.ckiT[:, :], [], [ki2])
        kb.cp("act", kiH2[:, :], ki2[:, :], [ki2], [kiH2])
        kb.tt(kiL2[:, :], ki2[:, :], kiH2[:, :], ALU.subtract, [ki2, kiH2], [kiL2])
        kb.dma("sp", kT[:, :, :], g.ckT.rearrange("(g p) s -> p g s", p=128), [], [kT])
        kb.dma("sp", vt[:, :, :], g.cv_tok.rearrange("(n p) c -> p n c", p=128), [], [vt])
        kb.dma("sp", wiT[:, :], g.cwiT[:, :], [], [wiT])
        ps = g.psum()
        kb.tr_multi([(ps[:, n * 16:(n + 1) * 16], wiT[0:16, n * 128:(n + 1) * 128]) for n in range(16)],
                    cst[0:16, C_IDENT:C_IDENT + 16], [wiT], [ps])
        kb.cp("act", witok[:, :, :], ps[:, 0:256].rearrange("p (a b) -> p a b", a=16), [ps], [witok])
        kb.act(wabs[:, :, :], witok[:, :, :], AF.Abs, [witok], [wabs])
        kb.act(wsgn[:, :, :], witok[:, :, :], AF.Sign, [witok], [wsgn])
        ksq = sb(nc, es, "dksq", [128, 4, S], BF16)
        kpart = sb(nc, es, "dkpart", [128, 16], F32)
        kmx = sb(nc, es, "dkmx", [128, 4], F32)
        qsqs = [sb(nc, es, "dqsq%d" % i, [128, 16, 128], BF16) for i in range(2)]
        negBs = [sb(nc, es, "dnegB%d" % i, [128, 16], F32) for i in range(2)]
        kb.act(ksq[:, :, :], kT[:, :, :], AF.Square, [kT], [ksq])
        for gk in range(4):
            for t4 in range(4):
                ps = g.psum()
                kb.mm(ps[:, :], [(g.onesb[:, :], ksq[:, gk, t4 * 512:(t4 + 1) * 512])], [ksq], [ps])
                kb.op("dve", lambda eng, ps=ps, o=kpart[:, gk * 4 + t4:gk * 4 + t4 + 1]: eng.tensor_reduce(out=o, in_=ps[:, :], axis=mybir.AxisListType.X, op=ALU.max), [ps], [kpart])
        kb.op("dve", lambda eng: eng.tensor_reduce(out=kmx[:, :], in_=kpart[:, :].rearrange("p (a b) -> p a b", a=4), axis=mybir.AxisListType.X, op=ALU.max), [kpart], [kmx])
        qiv = g.cqiT.rearrange("(h e) s -> e h s", e=64)
        qv = g.cqT.rearrange("(h p) s -> p h s", p=128)
        oTv = g.oT.rearrange("(h p) s -> p h s", p=128)
        scores = [score, score1]
        mbbs = [mbb, mbb1]

        def tiles_of(b):
            ncols = (b + 1) * 128
            return ncols, [(c0, min(512, ncols - c0)) for c0 in range(0, ncols, 512)]

        def indexer_units(b):
            qi, sc = qib[b % 2], scores[b % 2]
            ncols, tiles = tiles_of(b)
            units = []

            def load():
                kb.dma("sp", qi2[0:64, :, :], qiv[:, :, b * 128:(b + 1) * 128], [], [qi2])
                kb.dma("sp", qi2[64:128, :, :], qiv[:, :, b * 128:(b + 1) * 128], [], [qi2])
                kb.cp("act", qi[:, :, :], qi2[:, :, :], [qi2], [qi])
                kb.tt(qi[64:128, :, :], qi2[64:128, :, :], qi[64:128, :, :], ALU.subtract, [qi2, qi], [qi])
            units.append(load)
            for h in range(16):
                def u(h=h):
                    for (c0, w) in tiles:
                        ps = g.psum()
                        kb.mm(ps[:, 0:w], [(qi[:, h, :], kiH2[:, c0:c0 + w]), (qi[:, h, :], kiL2[:, c0:c0 + w])], [qi, kiH2, kiL2], [ps])
                        rl = rl_.next()
                        kb.act(rl[:, 0:w], ps[:, 0:w], AF.Relu, [ps, wabs], [rl], scale=wabs[:, b, h:h + 1])
                        if h == 0:
                            kb.ts(sc[:, c0:c0 + w], rl[:, 0:w], wsgn[:, b, 0:1], None, ALU.mult, None, [rl, wsgn], [sc])
                        else:
                            kb.stt(sc[:, c0:c0 + w], rl[:, 0:w], wsgn[:, b, h:h + 1], sc[:, c0:c0 + w], ALU.mult, ALU.add, [rl, wsgn, sc], [sc])
                units.append(u)

            def fin():
                kb.tt(sc[:, b * 128:(b + 1) * 128], sc[:, b * 128:(b + 1) * 128], CB, ALU.add, [sc], [sc])
            units.append(fin)
            return units

        def topk_units(b):
            sc, mbb_ = scores[b % 2], mbbs[b % 2]
            ncols, tiles = tiles_of(b)
            units = []
            if b >= 2:
                for r2 in range(16):
                    def u(r2=r2):
                        for r in (2 * r2, 2 * r2 + 1):
                            cur = sc if r == 0 else work
                            kb.op("dve", lambda eng, c=cur, n_=ncols: eng.max(out=m8[:, :], in_=c[:, 0:n_]), [cur], [m8])
                            if r < 31:
                                kb.op("dve", lambda eng, c=cur, n_=ncols: eng.match_replace(out=work[:, 0:n_], in_to_replace=m8[:, :], in_values=c[:, 0:n_], imm_value=NEG),
                                      [cur, m8], [work])
                    units.append(u)

            def fin():
                if b >= 2:
                    kb.ts(mb[:, 0:ncols], sc[:, 0:ncols], m8[:, 7:8], None, ALU.is_ge, None, [sc, m8], [mb])
                else:
                    kb.ts(mb[:, 0:ncols], sc[:, 0:ncols], -1.0e29, None, ALU.is_gt, None, [sc], [mb])
                kb.ts(mbb_[:, 0:ncols], mb[:, 0:ncols], -1.0, 1.0e30, ALU.add, ALU.mult, [mb], [mbb_])
            units.append(fin)
            return units

        def attn_block(b, hook):
            q, mbb_ = qb[b % 2], mbbs[b % 2]
            ncols, tiles = tiles_of(b)
            nt = len(tiles)
            kb.dma("sp", q[:, :, :], qv[:, :, b * 128:(b + 1) * 128], [], [q])
            qsq, negB = qsqs[b % 2], negBs[b % 2]
            kb.act(qsq[:, :, :], q[:, :, :], AF.Square, [q], [qsq])
            psn = g.psum()
            kb.mm_multi([(psn[:, 2 * h:2 * h + 2], [(qsq[:, h, :], g.onesb[:, 0:2])]) for h in range(16)], [qsq], [psn])
            kb.tt(negB[:, :].rearrange("p (a j) -> p a j", a=4),
                  psn[:, 0:32].rearrange("p (a j two) -> p a j two", a=4, two=2)[:, :, :, 0],
                  kmx[:, :].unsqueeze(2).to_broadcast([128, 4, 4]), ALU.mult, [psn, kmx], [negB])
            kb.act(negB[:, :], negB[:, :], AF.Sqrt, [negB], [negB])
            kb.ts(negB[:, :], negB[:, :], -1.0, None, ALU.mult, None, [negB], [negB])

            def stageA(h):
                gq = h // 4
                Pm, st = Pms[h % 2], sts[h % 2]
                pz = []
                for ti, (c0, w) in enumerate(tiles):
                    ps = g.psum()
                    kb.mm(ps[:, 0:w], [(q[:, h, :], kT[:, gq, c0:c0 + w]), (identb[:, :], mbb_[:, c0:c0 + w])], [q, kT, mbb_, identb], [ps])
                    pz.append(ps)
                for ti, (c0, w) in enumerate(tiles):
                    kb.act(Pm[:, c0:c0 + w], pz[ti][:, 0:w], AF.Exp, [pz[ti], negB], [Pm, st], bias=negB[:, h:h + 1], accum=st[:, 8 + ti:9 + ti])
                if nt > 1:
                    kb.op("dve", lambda eng, o=st[:, 2:3], i=st[:, 8:8 + nt]: eng.tensor_reduce(out=o, in_=i, axis=mybir.AxisListType.X, op=ALU.add), [st], [st])
                    rsum = st[:, 2:3]
                else:
                    rsum = st[:, 8:9]
                kb.op("dve", lambda eng, o=st[:, 3:4], i=rsum: eng.reciprocal(out=o, in_=i), [st], [st])

            def stageB(h):
                gq = h // 4
                Pm, PT, st = Pms[h % 2], PTs[h % 2], sts[h % 2]
                for k8 in range(0, b + 1, 8):
                    nk = min(8, b + 1 - k8)
                    ps = g.tslots[g.tsi % 2]
                    g.tsi += 1
                    kb.tr_multi([(ps[:, j * 128:(j + 1) * 128], Pm[:, (k8 + j) * 128:(k8 + j + 1) * 128]) for j in range(nk)], identb[:, :], [Pm, identb], [ps])
                    kb.cp("act", PT[:, k8:k8 + nk, :], ps[:, 0:nk * 128].rearrange("p (a b) -> p a b", a=nk), [ps], [PT])
                ps = g.psum()
                kb.mm(ps[:, 0:128], [(PT[:, k2, :], vt[:, k2, gq * 128:(gq + 1) * 128]) for k2 in range(b + 1)], [PT, vt], [ps])
                kb.act(oblk[:, h, :], ps[:, 0:128], AF.Copy, [ps, st], [oblk], scale=st[:, 3:4])
            stageA(0)
            for h in range(16):
                if h + 1 < 16:
                    stageA(h + 1)
                stageB(h)
                hook()
            for h4 in range(4):
                ps = g.psum()
                kb.tr_multi([(ps[:, j * 128:(j + 1) * 128], oblk[:, h4 * 4 + j, :]) for j in range(4)], ident, [oblk], [ps])
                kb.cp("act", oTb[:, h4 * 4:(h4 + 1) * 4, :], ps[:, :].rearrange("p (a b) -> p a b", a=4), [ps], [oTb])
            kb.dma("pool", oTv[:, :, b * 128:(b + 1) * 128], oTb[:, :, :], [oTb], [])

        for u in indexer_units(0):
            u()
        for u in topk_units(0):
            u()
        for u in indexer_units(1):
            u()
        for b in range(16):
            lists = []
            if b + 1 < 16:
                lists.append(topk_units(b + 1))
            if b + 2 < 16:
                lists.append(indexer_units(b + 2))
            pos = [0] * len(lists)
            per = [-(-len(l) // 16) for l in lists]

            def hook():
                for li, l in enumerate(lists):
                    for _ in range(per[li]):
                        if pos[li] < len(l):
                            l[pos[li]]()
                            pos[li] += 1
            attn_block(b, hook)
            for li, l in enumerate(lists):
                while pos[li] < len(l):
                    l[pos[li]]()
                    pos[li] += 1
        kb.barrier()


def phase_final_norm(g, xsrc, gain_ap, dst):
    kb, nc = g.kb, g.nc
    with ExitStack() as es:
        xs = [sb(nc, es, "fx%d" % i, [128, 16, 512], F32) for i in range(2)]
        sq = sb(nc, es, "fsq", [128, 16, 512], BF16)
        ob = sb(nc, es, "fob", [128, 16, 512], F32)
        rs = sb(nc, es, "frs", [128, 512], F32)
        xv = xsrc.rearrange("(c p) s -> p c s", p=128)
        dv = dst.rearrange("(c p) s -> p c s", p=128)
        for t in range(4):
            x = xs[t % 2]
            for q4 in range(4):
                kb.dma("sp", x[:, q4 * 4:(q4 + 1) * 4, :], xv[:, q4 * 4:(q4 + 1) * 4, t * 512:(t + 1) * 512], [], [x])
            kb.act(sq[:, :, :], x[:, :, :], AF.Square, [x], [sq])
            ps = g.psum()
            kb.mm(ps[:, :], [(g.onesb[:, :], sq[:, c, :]) for c in range(16)], [sq], [ps])
            kb.act(rs[:, :], ps[:, :], AF.Ln, [ps], [rs], bias=g.eps[:, 0:1], scale=1.0 / D)
            kb.act(rs[:, :], rs[:, :], AF.Exp, [rs], [rs], scale=-0.5)
            for c in range(16):
                kb.stt(ob[:, c, :], x[:, c, :], gain_ap[:, c:c + 1], rs[:, :], ALU.mult, ALU.mult, [x, rs], [ob])
            for q4 in range(4):
                kb.dma("pool", dv[:, q4 * 4:(q4 + 1) * 4, t * 512:(t + 1) * 512], ob[:, q4 * 4:(q4 + 1) * 4, :], [ob], [])
        kb.barrier()


def build_program(stop_after=None, dumps=(), skip_l0=False):
    nc = bass.Bass("TRN2", target_bir_lowering=False)
    g = Ctx()
    g.nc = nc

    def din(name, shape, dt=F32):
        return nc.dram_tensor(name, list(shape), dt, kind="ExternalInput").ap()

    def dscr(name, shape, dt=F32):
        return nc.dram_tensor(name, list(shape), dt).ap()
    xT = din("xT", [D, S])
    pos = din("pos", [1, S], I32)
    nmix = din("nmix", [128, 32])
    nffn = din("nffn", [128, 32])
    fnorm = din("fnorm", [128, 16])
    g.w_in_ab = din("w_in_ab", [D, AB_IN])
    convw = din("convw", [128, 96])
    alog = din("alog", [8, 1])
    dtb = din("dtb", [8, 1])
    gnorm = din("gnorm", [128, 1])
    g.w_out_ab = din("w_out_ab", [D, D])
    g.w_in_c = din("w_in_c", [D, C_IN])
    g.w_out_c = din("w_out_c", [D, D])
    g.ffn_gate = din("ffn_gate", [2 * D, DFF])
    g.ffn_up = din("ffn_up", [2 * D, DFF])
    g.ffn_down = din("ffn_down", [2 * DFF, D])
    consts = din("consts", [128, NCONST])
    outT = nc.dram_tensor("outT", [D, S], F32, kind="ExternalOutput").ap()
    dump_aps = {}
    for (nm, shape, dt) in dumps:
        dump_aps[nm] = nc.dram_tensor("dump_" + nm, list(shape), dt, kind="ExternalOutput").ap()

    def scr(name, shape, dt=F32):
        if name in dump_aps:
            return dump_aps[name]
        return dscr(name, shape, dt)
    g.xres = scr("xres", [D, S])
    g.P0T = scr("P0T", [4112, S])
    g.bqT = scr("bqT", [1024, S], BF16)
    g.bkT = scr("bkT", [1024, S], BF16)
    g.bv_tok = scr("bv_tok", [S, 1024], BF16)
    g.gqT = scr("gqT", [1024, S], BF16)
    g.gkT = scr("gkT", [1024, S], BF16)
    g.gk_tok = scr("gk_tok", [S, 1024])
    g.gv_tok = scr("gv_tok", [S, 1024])
    g.oT = scr("oT", [D, S], BF16)
    g.actT = scr("actT", [DFF, S], BF16)
    g.cqT = scr("cqT", [2048, S], BF16)
    g.ckT = scr("ckT", [512, S], BF16)
    g.cv_tok = scr("cv_tok", [S, 512], BF16)
    g.cqiT = scr("cqiT", [1024, S])
    g.ckiT = scr("ckiT", [64, S])
    g.cwiT = scr("cwiT", [16, S])

    with ExitStack() as es:
        kb = KB(nc, es)
        g.kb = kb
        cst = sb(nc, es, "cst", [128, NCONST], F32)
        g.cst = cst.t
        g.ones = cst.t[:, C_ONES:C_ONES + 128]
        small = sb(nc, es, "csm", [128, 4], F32)
        g.eps = small.t[:, 0:1]
        g.one = small.t[:, 1:2]
        gains = sb(nc, es, "gains", [128, 80], F32)
        cw = sb(nc, es, "convw", [128, 96], F32)
        g.convw = cw.t
        sm8 = sb(nc, es, "sm8", [8, 2], F32)
        g.alog = sm8.t[:, 0:1]
        g.dtb = sm8.t[:, 1:2]
        gn = sb(nc, es, "gn", [128, 1], F32)
        g.gnorm = gn.t
        ps2 = [es.enter_context(nc.psum_tensor("ps%d" % i, [128, 1024], F32)) for i in range(3)]
        psb = es.enter_context(nc.psum_tensor("psb", [128, 2048], BF16))
        banks = []
        for i in range(6):
            t = Tl(ps2[i // 2])
            t.t = ps2[i // 2][:, (i % 2) * 512:(i % 2 + 1) * 512]
            banks.append(t)
        g.tslots = []
        for i in range(2):
            t = Tl(psb)
            t.t = psb[:, i * 1024:(i + 1) * 1024]
            g.tslots.append(t)
        g.ring = banks
        g.pi = 0

        def psum():
            t = g.ring[g.pi % len(g.ring)]
            g.pi += 1
            return t
        g.psum = psum
        kb.dma("sp", cst[:, :], consts[:, :], [], [cst])
        kb.dma("sp", gains[:, 0:32], nmix[:, :], [], [gains])
        kb.dma("sp", gains[:, 32:64], nffn[:, :], [], [gains])
        kb.dma("sp", gains[:, 64:80], fnorm[:, :], [], [gains])
        kb.dma("sp", cw[:, :], convw[:, :], [], [cw])
        kb.dma("sp", sm8[:, 0:1], alog[:, :], [], [sm8])
        kb.dma("sp", sm8[:, 1:2], dtb[:, :], [], [sm8])
        kb.dma("sp", gn[:, :], gnorm[:, :], [], [gn])
        onesb = sb(nc, es, "onesb", [128, 128], BF16)
        g.onesb = onesb.t
        rqb = sb(nc, es, "rqb", [128, 128], BF16)
        rib = sb(nc, es, "rib", [128, 128], BF16)
        g.rqb, g.rib = rqb.t, rib.t
        kb.cp("dve", onesb[:, :], cst[:, C_ONES:C_ONES + 128], [cst], [onesb])
        kb.cp("dve", rqb[:, :], cst[:, C_RQ:C_RQ + 128], [cst], [rqb])
        kb.cp("dve", rib[:, :], cst[:, C_RI:C_RI + 128], [cst], [rib])
        kb.op("dve", lambda eng: eng.memset(small[:, 0:1], EPS), [], [small])
        kb.op("dve", lambda eng: eng.memset(small[:, 1:2], 1.0), [], [small])
        for q4 in range(4):
            kb.dma("sp", g.xres[q4 * 512:(q4 + 1) * 512, :], xT[q4 * 512:(q4 + 1) * 512, :], [], [])
        kb.barrier()

        def run_l0():
            with ExitStack() as es1:
                hT = sb(nc, es1, "hT", [128, 16, S], BF16)
                phase_norm(g, g.xres, gains.t[:, 0:16], hT)
                phase_inproj0(g, hT)
            if stop_after == "inproj0":
                return
            with ExitStack() as es1:
                gtok = sb(nc, es1, "gtok", [64, 32, 8], F32)
                btok = sb(nc, es1, "btok", [64, 32, 8], F32)
                phase_gdn_pre(g, gtok, btok)
                if stop_after == "gdn_pre":
                    return
                phase_gdn_main(g, gtok, btok)
            if stop_after == "gdn":
                return
            phase_sb(g)
            if stop_after == "sb":
                return
            with ExitStack() as es1:
                oTs = sb(nc, es1, "oTs", [128, 16, S], BF16)
                ov = g.oT.rearrange("(c p) s -> p c s", p=128)
                for q4 in range(4):
                    kb.dma("sp", oTs[:, q4 * 4:(q4 + 1) * 4, :], ov[:, q4 * 4:(q4 + 1) * 4, :], [], [oTs])
                phase_residual_dense(g, oTs, 16, 0, S, g.w_out_ab, g.xres, 512)
            if stop_after == "x1":
                return
            with ExitStack() as es1:
                hT = sb(nc, es1, "hT", [128, 16, S], BF16)
                phase_norm(g, g.xres, gains.t[:, 32:48], hT)
                phase_ffn(g, 0, hT)
            phase_ffn2(g, 0)
            if stop_after == "x2":
                return

        def run():
            if not skip_l0:
                run_l0()
                if stop_after in ("inproj0", "gdn_pre", "gdn", "sb", "x1", "x2"):
                    return
            with ExitStack() as es1:
                hT = sb(nc, es1, "hT", [128, 16, S], BF16)
                if not skip_l0:
                    phase_norm(g, g.xres, gains.t[:, 16:32], hT)
                else:
                    phase_norm(g, xT, gains.t[:, 16:32], hT)
                tabs = build_rope_tables(g, es1, pos)
                phase_inproj1(g, hT, tabs)
            if stop_after == "inproj1":
                return
            phase_dsa(g)
            if stop_after == "dsa":
                return
            with ExitStack() as es1:
                oTs = sb(nc, es1, "oTs", [128, 16, S], BF16)
                ov = g.oT.rearrange("(c p) s -> p c s", p=128)
                for q4 in range(4):
                    kb.dma("sp", oTs[:, q4 * 4:(q4 + 1) * 4, :], ov[:, q4 * 4:(q4 + 1) * 4, :], [], [oTs])
                phase_residual_dense(g, oTs, 16, 0, S, g.w_out_c, g.xres, 512)
            if stop_after == "x3":
                return
            with ExitStack() as es1:
                hT = sb(nc, es1, "hT", [128, 16, S], BF16)
                phase_norm(g, g.xres, gains.t[:, 48:64], hT)
                phase_ffn(g, 1, hT)
            phase_ffn2(g, 1)
        run()
        kb.barrier()
        if stop_after is None:
            phase_final_norm(g, g.xres, gains.t[:, 64:80], outT)
        else:
            for q4 in range(4):
                kb.dma("sp", outT[q4 * 512:(q4 + 1) * 512, :], g.xres[q4 * 512:(q4 + 1) * 512, :], [], [])
        kb.barrier()
        block = es.enter_context(nc.Block())

        @block.tensor
        def _(e):
            for th in kb.thunks["pe"]:
                th(e)

        @block.vector
        def _(e):
            for th in kb.thunks["dve"]:
                th(e)

        @block.scalar
        def _(e):
            for th in kb.thunks["act"]:
                th(e)

        @block.gpsimd
        def _(e):
            for th in kb.thunks["pool"]:
                th(e)

        @block.sync
        def _(e):
            for th in kb.thunks["sp"]:
                th(e)
        print("[kernel] recorded ops:", kb.ninst, {e: len(kb.thunks[e]) for e in ENGS})
    return nc


def make_in_maps(inputs):
    consts = build_consts()
    shared = {
        "nmix": np.ascontiguousarray(inputs["norm_mix"].reshape(2, 16, 128).transpose(2, 0, 1).reshape(128, 32)),
        "nffn": np.ascontiguousarray(inputs["norm_ffn"].reshape(2, 16, 128).transpose(2, 0, 1).reshape(128, 32)),
        "fnorm": np.ascontiguousarray(inputs["final_norm"].reshape(16, 128).T),
        "w_in_ab": np.ascontiguousarray(inputs["w_in_ab"][0]),
        "convw": np.ascontiguousarray(inputs["conv_w_a"][0].reshape(4, 24, 128).transpose(2, 1, 0).reshape(128, 96)),
        "alog": np.ascontiguousarray(inputs["a_log"][0].reshape(8, 1)),
        "dtb": np.ascontiguousarray(inputs["dt_bias"][0].reshape(8, 1)),
        "gnorm": np.ascontiguousarray(inputs["gdn_norm"][0].reshape(128, 1)),
        "w_out_ab": np.ascontiguousarray(inputs["w_out_ab"][0]),
        "w_in_c": np.ascontiguousarray(inputs["w_in_c"][0]),
        "w_out_c": np.ascontiguousarray(inputs["w_out_c"][0]),
        "ffn_gate": np.ascontiguousarray(inputs["ffn_gate"].reshape(2 * D, DFF)),
        "ffn_up": np.ascontiguousarray(inputs["ffn_up"].reshape(2 * D, DFF)),
        "ffn_down": np.ascontiguousarray(inputs["ffn_down"].reshape(2 * DFF, D)),
        "consts": consts,
    }
    shared = {k: np.asarray(v, np.float32) for k, v in shared.items()}
    maps = []
    for b in range(8):
        m = dict(shared)
        m["xT"] = np.ascontiguousarray(np.asarray(inputs["x"][b], np.float32).T)
        m["pos"] = np.ascontiguousarray(np.asarray(inputs["positions"][b], np.int32).reshape(1, S))
        maps.append(m)
    return maps


def kernel(**inputs):
    nc = build_program()
    maps = make_in_maps(inputs)
    res = run_bass_kernel_spmd(nc, maps, core_ids=list(range(8)))
    out = np.stack([np.ascontiguousarray(r["outT"].T) for r in res.results], axis=0)
    return out.astype(np.float32)
```
